# Optimizing a Trainium2 kernel written in Bass

```python
import jax, jax.numpy as jnp
from jax import lax
import numpy as np

D_MODEL = 1024
BATCH = 8
SEQ = 4096
DEPTH = 1

N_META = 16
LRU_WIDTH = D_MODEL // 2
LRU_HEADS = 8
LRU_HEAD_DIM = LRU_WIDTH // LRU_HEADS
CONV_WIDTH = 4
LRU_C = 8.0
POOL_WIDTH = D_MODEL - LRU_WIDTH
POOL_WINDOWS = (2, 4, 8, 16)
POOL_GROUP = POOL_WIDTH // len(POOL_WINDOWS)
MIX_WIDTH = LRU_WIDTH + POOL_WIDTH
IN_WIDTH = 2 * LRU_WIDTH + POOL_WIDTH
N_GROUPS = 4
EXPERTS_PER_GROUP = 8
N_EXPERTS = N_GROUPS * EXPERTS_PER_GROUP
TOP_K = 2
EXPERT_FF = D_MODEL // 2
MOE_BLOCK = 128
RMS_EPS = 1e-6

kernel_name = "hymba_rglru_multipool_hmoe_layer"


def rmsnorm(x, gain):
    xf = x.astype(jnp.float32)
    y = xf * lax.rsqrt(jnp.mean(xf * xf, axis=-1, keepdims=True) + RMS_EPS)
    return (y * gain.astype(jnp.float32)).astype(x.dtype)


def rg_lru_group(u_x, u_gate, conv_w, conv_b, wa, ba, wx, bx, lam):
    B, T, _ = u_x.shape
    xpad = jnp.pad(u_x, ((0, 0), (CONV_WIDTH - 1, 0), (0, 0)))
    xc = conv_b + sum(xpad[:, k:k + T] * conv_w[k] for k in range(CONV_WIDTH))
    xf = xc.astype(jnp.float32)
    xh = xf.reshape(B, T, LRU_HEADS, LRU_HEAD_DIM)
    r = jax.nn.sigmoid(jnp.einsum('bthi,hij->bthj', xh, wa.astype(jnp.float32)).reshape(B, T, LRU_WIDTH)
                       + ba.astype(jnp.float32))
    i = jax.nn.sigmoid(jnp.einsum('bthi,hij->bthj', xh, wx.astype(jnp.float32)).reshape(B, T, LRU_WIDTH)
                       + bx.astype(jnp.float32))
    log_a = LRU_C * r * jax.nn.log_sigmoid(lam.astype(jnp.float32))
    a = jnp.exp(log_a)
    b = jnp.sqrt(-jnp.expm1(2.0 * log_a)) * (i * xf)

    def combine(left, right):
        a1, b1 = left
        a2, b2 = right
        return a1 * a2, a2 * b1 + b2

    _, h = lax.associative_scan(combine, (a, b), axis=1)
    return h * jax.nn.gelu(u_gate.astype(jnp.float32))


def multiscale_pool_group(u, pool_w):
    uf = u.astype(jnp.float32)
    T = uf.shape[1]
    count_base = jnp.arange(1, T + 1, dtype=jnp.float32)[None, :, None]
    outs = []
    for g, w in enumerate(POOL_WINDOWS):
        ug = uf[..., g * POOL_GROUP:(g + 1) * POOL_GROUP]
        cs = jnp.cumsum(ug, axis=1)
        lag = jnp.pad(cs[:, :T - w], ((0, 0), (w, 0), (0, 0)))
        mean = (cs - lag) / jnp.minimum(count_base, float(w))
        outs.append(jnp.einsum('btc,cd->btd', mean - ug, pool_w[g].astype(jnp.float32)))
    return jnp.concatenate(outs, axis=-1)


def hierarchical_moe(h, w_group, b_group, w_router, b_router, w_gate, w_up, w_down):
    B, T, D = h.shape
    N = B * T
    xt = h.reshape(N, D)
    g_logits = (xt @ w_group).astype(jnp.float32) + b_group.astype(jnp.float32)
    p_groups = jax.nn.softmax(g_logits, axis=-1)
    g_idx = jnp.argmax(g_logits, axis=-1).astype(jnp.int32)
    p_g = jnp.take_along_axis(p_groups, g_idx[:, None], axis=-1)
    e_logits = ((xt @ w_router).astype(jnp.float32) + b_router.astype(jnp.float32)
                ).reshape(N, N_GROUPS, EXPERTS_PER_GROUP)
    e_sel = jnp.take_along_axis(e_logits, g_idx[:, None, None], axis=1)[:, 0]
    top_v, top_i = lax.top_k(e_sel, TOP_K)
    gate = p_g * jax.nn.softmax(top_v, axis=-1)
    eid = g_idx[:, None] * EXPERTS_PER_GROUP + top_i.astype(jnp.int32)

    A = N * TOP_K
    flat_e = eid.reshape(A)
    flat_tok = jnp.repeat(jnp.arange(N, dtype=jnp.int32), TOP_K)
    flat_gate = gate.reshape(A)
    order = jnp.argsort(flat_e)
    se, stok, sg = flat_e[order], flat_tok[order], flat_gate[order]
    counts = jnp.bincount(flat_e, length=N_EXPERTS)
    starts = jnp.cumsum(counts) - counts
    padded = ((counts + MOE_BLOCK - 1) // MOE_BLOCK) * MOE_BLOCK
    pends = jnp.cumsum(padded)
    pstarts = pends - padded
    dest = pstarts[se] + (jnp.arange(A, dtype=jnp.int32) - starts[se])
    n_blocks = -(-(A + N_EXPERTS * (MOE_BLOCK - 1)) // MOE_BLOCK)
    P = n_blocks * MOE_BLOCK
    buf = jnp.zeros((P, D), h.dtype).at[dest].set(xt[stok])
    block_e = jnp.clip(jnp.searchsorted(pends, jnp.arange(n_blocks) * MOE_BLOCK, side='right'),
                       0, N_EXPERTS - 1).astype(jnp.int32)

    def expert_block(args):
        xb, e = args
        hid = jax.nn.silu(xb @ w_gate[e]) * (xb @ w_up[e])
        return hid @ w_down[e]

    out = lax.map(expert_block, (buf.reshape(n_blocks, MOE_BLOCK, D), block_e)).reshape(P, D)
    contrib = (out[dest].astype(jnp.float32) * sg[:, None]).astype(h.dtype)
    y = jnp.zeros((N, D), h.dtype).at[stok].add(contrib)
    return y.reshape(B, T, D)


def setup_inputs(seed: int = 0) -> dict:
    key = jax.random.key(seed)
    ks = jax.random.split(key, 24)
    f32 = jnp.float32

    def nrm(k, shape, scale):
        return jax.random.normal(k, shape, f32) * scale

    u = jax.random.uniform(ks[10], (DEPTH, LRU_WIDTH), f32, minval=0.9, maxval=0.999)
    s = u ** (1.0 / LRU_C)
    lam = jnp.log(s) - jnp.log1p(-s)
    return {
        "x": nrm(ks[0], (BATCH, SEQ, D_MODEL), 1.0),
        "meta_tokens": nrm(ks[1], (N_META, D_MODEL), 1.0),
        "norm1_gain": 1.0 + nrm(ks[2], (DEPTH, D_MODEL), 0.05),
        "w_in": nrm(ks[3], (DEPTH, D_MODEL, IN_WIDTH), D_MODEL ** -0.5),
        "conv_w": nrm(ks[4], (DEPTH, CONV_WIDTH, LRU_WIDTH), CONV_WIDTH ** -0.5),
        "conv_b": nrm(ks[5], (DEPTH, LRU_WIDTH), 0.02),
        "lru_wa": nrm(ks[6], (DEPTH, LRU_HEADS, LRU_HEAD_DIM, LRU_HEAD_DIM), LRU_HEAD_DIM ** -0.5),
        "lru_ba": nrm(ks[7], (DEPTH, LRU_WIDTH), 0.02),
        "lru_wx": nrm(ks[8], (DEPTH, LRU_HEADS, LRU_HEAD_DIM, LRU_HEAD_DIM), LRU_HEAD_DIM ** -0.5),
        "lru_bx": nrm(ks[9], (DEPTH, LRU_WIDTH), 0.02),
        "lru_lambda": lam,
        "lru_out_gain": 1.0 + nrm(ks[11], (DEPTH, LRU_WIDTH), 0.05),
        "pool_w": nrm(ks[12], (DEPTH, len(POOL_WINDOWS), POOL_GROUP, POOL_GROUP), POOL_GROUP ** -0.5),
        "pool_scale": 1.0 + nrm(ks[13], (DEPTH, POOL_WIDTH), 0.1),
        "w_out": nrm(ks[14], (DEPTH, MIX_WIDTH, D_MODEL), MIX_WIDTH ** -0.5),
        "norm2_gain": 1.0 + nrm(ks[15], (DEPTH, D_MODEL), 0.05),
        "w_group": nrm(ks[16], (DEPTH, D_MODEL, N_GROUPS), D_MODEL ** -0.5),
        "b_group": nrm(ks[17], (DEPTH, N_GROUPS), 0.01),
        "w_router": nrm(ks[18], (DEPTH, D_MODEL, N_EXPERTS), D_MODEL ** -0.5),
        "b_router": nrm(ks[19], (DEPTH, N_EXPERTS), 0.01),
        "w_gate": nrm(ks[20], (DEPTH, N_EXPERTS, D_MODEL, EXPERT_FF), D_MODEL ** -0.5),
        "w_up": nrm(ks[21], (DEPTH, N_EXPERTS, D_MODEL, EXPERT_FF), D_MODEL ** -0.5),
        "w_down": nrm(ks[22], (DEPTH, N_EXPERTS, EXPERT_FF, D_MODEL), EXPERT_FF ** -0.5),
        "final_gain": 1.0 + nrm(ks[23], (D_MODEL,), 0.05),
    }


def reference(x, meta_tokens, norm1_gain, w_in, conv_w, conv_b, lru_wa, lru_ba, lru_wx, lru_bx,
              lru_lambda, lru_out_gain, pool_w, pool_scale, w_out, norm2_gain, w_group, b_group,
              w_router, b_router, w_gate, w_up, w_down, final_gain):
    B = x.shape[0]
    meta = jnp.broadcast_to(meta_tokens[None].astype(x.dtype), (B, N_META, D_MODEL))
    h = jnp.concatenate([meta, x], axis=1)
    for l in range(DEPTH):
        hn = rmsnorm(h, norm1_gain[l])
        proj = hn @ w_in[l]
        u_x = proj[..., :LRU_WIDTH]
        u_g = proj[..., LRU_WIDTH:2 * LRU_WIDTH]
        u_p = proj[..., 2 * LRU_WIDTH:]
        y_lru = rmsnorm(rg_lru_group(u_x, u_g, conv_w[l], conv_b[l], lru_wa[l], lru_ba[l],
                                     lru_wx[l], lru_bx[l], lru_lambda[l]), lru_out_gain[l])
        y_pool = rmsnorm(multiscale_pool_group(u_p, pool_w[l]), pool_scale[l])
        y = jnp.concatenate([y_lru, y_pool], axis=-1).astype(h.dtype) @ w_out[l]
        h = h + y
        hn = rmsnorm(h, norm2_gain[l])
        h = h + hierarchical_moe(hn, w_group[l], b_group[l], w_router[l], b_router[l],
                                 w_gate[l], w_up[l], w_down[l])
    h = rmsnorm(h, final_gain)
    return h[:, N_META:, :]
```

```python
import contextlib
import numpy as np
import concourse.bass as bass
import concourse.mybir as mybir
from concourse.bass_utils import run_bass_kernel_spmd

F32 = mybir.dt.float32
BF16 = mybir.dt.bfloat16
I32 = mybir.dt.int32
AF = mybir.ActivationFunctionType
ALU = mybir.AluOpType
AX = mybir.AxisListType

D = 1024
NX = 4096
NM = 16
W = 512
H = 16
NCH = NX // W
NE = 32
CAP = 512
NB = CAP // 128
FF = 512
INW = 1536
EPS = 1e-6
NT = NX // 128
NCORES = 8
GRAN = 1


class Sched:
    ENGS = ("pe", "act", "dve", "pool", "sp")

    def __init__(self, nc, stack, n_dma_sems=64):
        self.nc = nc
        self.ops = {e: [] for e in self.ENGS}
        self.seq = {e: 0 for e in self.ENGS}
        self.waited = {e: {} for e in self.ENGS}
        self.last_w = {}
        self.readers = {}
        self.esem = {e: stack.enter_context(nc.semaphore("s_" + e)) for e in self.ENGS if e != "sp"}
        self.dsem = [stack.enter_context(nc.semaphore("d_%d" % i)) for i in range(n_dma_sems)]
        self.duse = [0] * n_dma_sems
        half = n_dma_sems // 2
        self.ring = {"sw": list(range(0, half)), "hw": list(range(half, n_dma_sems))}
        self.rnext = {"sw": 0, "hw": 0}
        self.barrier_tokens = []

    def _sem(self, key):
        return self.esem[key] if isinstance(key, str) else self.dsem[key]

    def _deps(self, eng, reads, writes, extra=()):
        deps = list(extra) + self.barrier_tokens
        for r in reads:
            t = self.last_w.get(r)
            if t is not None:
                deps.append(t)
        for w in writes:
            t = self.last_w.get(w)
            if t is not None:
                deps.append(t)
            deps.extend(self.readers.get(w, ()))
        waits = {}
        for (k, v) in deps:
            if eng == "pe" and k == "pe":
                continue
            if self.waited[eng].get(k, 0) >= v:
                continue
            if waits.get(k, 0) < v:
                waits[k] = v
        for k, v in waits.items():
            self.waited[eng][k] = v
        return list(waits.items())

    def _commit(self, tok, reads, writes):
        for r in reads:
            self.readers.setdefault(r, []).append(tok)
        for w in writes:
            self.last_w[w] = tok
            self.readers[w] = []

    def op(self, eng, fn, reads=(), writes=()):
        waits = self._deps(eng, reads, writes)
        self.seq[eng] += 1
        tok = (eng, self.seq[eng])
        self.ops[eng].append((waits, fn, self.esem[eng], 1))
        self._commit(tok, reads, writes)
        return tok

    def dma(self, q, fn, reads=(), writes=()):
        rk = "sw" if q == "pool" else "hw"
        j = self.ring[rk][self.rnext[rk]]
        self.rnext[rk] = (self.rnext[rk] + 1) % len(self.ring[rk])
        extra = []
        if self.duse[j] > 0:
            extra.append((j, 16 * self.duse[j]))
        waits = self._deps(q, reads, writes, extra)
        self.duse[j] += 1
        tok = (j, 16 * self.duse[j])
        self.ops[q].append((waits, fn, self.dsem[j], 16))
        self._commit(tok, reads, writes)
        return tok

    def all_tokens(self):
        toks = [(j, 16 * u) for j, u in enumerate(self.duse) if u > 0]
        toks += [(e, self.seq[e]) for e in self.ENGS if e != "sp" and self.seq[e] > 0]
        return toks

    def barrier(self):
        self.barrier_tokens = self.all_tokens()

    def emit(self, final=False):
        nc = self.nc
        fin = []
        if final:
            for (k, v) in self.all_tokens():
                if self.waited["sp"].get(k, 0) < v:
                    fin.append((k, v))
        sched = self
        ops = self.ops
        self.ops = {e: [] for e in self.ENGS}

        def run(engname, eng):
            if engname == "pool":
                sched.bcreg = eng.to_reg(NE * CAP - 1)
            for (waits, fn, sem, inc) in ops[engname]:
                for (k, v) in waits:
                    eng.wait_ge(sched._sem(k), v)
                fn(eng).then_inc(sem, inc)
            if engname == "sp":
                for (k, v) in fin:
                    eng.wait_ge(sched._sem(k), v)

        with nc.Block() as block:
            @block.tensor
            def _(eng):
                run("pe", eng)

            @block.scalar
            def _(eng):
                run("act", eng)

            @block.vector
            def _(eng):
                run("dve", eng)

            @block.gpsimd
            def _(eng):
                run("pool", eng)

            @block.sync
            def _(eng):
                run("sp", eng)


def build(debug=False):
    nc = bass.Bass("TRN2", target_bir_lowering=False)

    def din(name, shape, dtype=F32):
        return nc.dram_tensor(name, shape, dtype, kind="ExternalInput").ap()

    x = din("x", [NX, D])
    meta = din("meta", [NM, D])
    w_in = din("w_in", [D, INW])
    w_out = din("w_out", [D, D])
    wgate = din("w_gate", [NE, D, FF])
    wup = din("w_up", [NE, D, FF])
    wdown = din("w_down", [NE, FF, D])
    vecs = din("vecs", [128, 56])
    g2bc_d = din("g2bc", [128, 8 * 128])
    bc = din("bc", [128, 3 * D + 36])
    wa_bd = din("wa_bd", [4, 128, 128])
    wx_bd = din("wx_bd", [4, 128, 128])
    pool_w = din("pool_w", [4, 128, 128])
    wr = din("wr", [D, 36])
    cst = din("cst", [128, 480])
    out = nc.dram_tensor("out", [NX, D], F32, kind="ExternalOutput").ap()
    dk = "ExternalOutput" if debug else "Internal"
    h1buf = nc.dram_tensor("h1buf", [NX, D], F32, kind=dk).ap()
    xbuf = nc.dram_tensor("xbuf", [NE * CAP, D], BF16, kind=dk).ap()
    obuf = nc.dram_tensor("obuf", [NE * CAP, D], BF16, kind=dk).ap()
    if debug:
        dbg_route = nc.dram_tensor("dbg_route", [128, 4 * NT], F32, kind="ExternalOutput").ap()

    with contextlib.ExitStack() as gst:
        S = Sched(nc, gst)

        defer = {"list": None}

        def est_cost(eng, method, n):
            if eng == "pe":
                return max(64, n) / 2400.0 + 0.02
            if eng == "act":
                return 0.22 + n / 1150.0
            if eng == "dve":
                f = {"scalar_tensor_tensor": 1.6, "tensor_tensor_scan": 2.4, "reciprocal": 6.0}.get(method, 1.0)
                return 0.15 + f * n / 960.0
            return 0.25 + n / 500.0

        def OP(eng, method, reads, writes, *args, **kw):
            fn = lambda e: getattr(e, method)(*args, **kw)
            if defer["list"] is not None:
                o = kw.get("out", args[0] if args else None)
                n = 1
                for d in list(o.shape)[1:]:
                    n *= int(d)
                defer["list"].append(("op", eng, fn, list(reads), list(writes), est_cost(eng, method, n)))
            else:
                S.op(eng, fn, reads, writes)

        def SDMA(q, fn, reads, writes):
            if defer["list"] is not None:
                defer["list"].append(("dma", q, fn, list(reads), list(writes), 1.1 if q == "pool" else 0.1))
            else:
                S.dma(q, fn, reads, writes)

        def DMA(q, reads, writes, **kw):
            SDMA(q, lambda e: e.dma_start(**kw), reads, writes)

        def record(f, *a):
            defer["list"] = []
            f(*a)
            out_l = defer["list"]
            defer["list"] = None
            return out_l

        sim = {"free": {e: 0.0 for e in Sched.ENGS}, "wfin": {}, "rfin": {}}

        def commit(*lists):
            lists = [l for l in lists if l]
            pos = [0] * len(lists)
            total = sum(len(l) for l in lists)
            free, wfin, rfin = sim["free"], sim["wfin"], sim["rfin"]
            for _ in range(total):
                best = None
                for k, l in enumerate(lists):
                    if pos[k] >= len(l):
                        continue
                    kind, q, fn, reads, writes, cost = l[pos[k]]
                    ready = 0.0
                    for r in reads:
                        ready = max(ready, wfin.get(r, 0.0))
                    for w in writes:
                        ready = max(ready, wfin.get(w, 0.0), rfin.get(w, 0.0))
                    start = max(free[q], ready + 0.2)
                    cand = (start, pos[k] / len(l), k)
                    if best is None or cand < best:
                        best = cand
                start, _, k = best
                kind, q, fn, reads, writes, cost = lists[k][pos[k]]
                pos[k] += 1
                free[q] = start + cost
                fin = start + cost + (3.0 if kind == "dma" else 0.0)
                for r in reads:
                    rfin[r] = max(rfin.get(r, 0.0), fin)
                for w in writes:
                    wfin[w] = fin
                    rfin[w] = 0.0
                if kind == "op":
                    S.op(q, fn, reads, writes)
                else:
                    S.dma(q, fn, reads, writes)

        def SB(stack, name, shape, dtype):
            return stack.enter_context(nc.sbuf_tensor(name, shape, dtype))

        def PS(stack, name, shape, dtype):
            return stack.enter_context(nc.psum_tensor(name, shape, dtype))

        cst_b = SB(gst, "cst_b", [128, 384], BF16)
        cst_f = SB(gst, "cst_f", [128, 96], F32)
        D0f = SB(gst, "D0f", [128, NT], F32)
        D1f = SB(gst, "D1f", [128, NT], F32)
        D0i = [SB(gst, "D0i%d" % i, [128, 1], I32) for i in range(NT)]
        D1i = [SB(gst, "D1i%d" % i, [128, 1], I32) for i in range(NT)]
        G0 = SB(gst, "G0", [128, NT], F32)
        G1 = SB(gst, "G1", [128, NT], F32)
        ident = cst_b[:, 0:128]
        tri = cst_b[:, 128:256]
        ones = cst_b[:, 256:384]
        iotacap = cst_f[:, 0:32]
        rec = cst_f[:, 32:96]

        DMA("pool", [], ["cst_b"], out=cst_b[:], in_=cst[:, 0:384])
        DMA("sp", [], ["cst_f"], out=cst_f[:], in_=cst[:, 384:480])

        def rstd_chain(ss_ap, key, scale):
            OP("dve", "tensor_scalar", [key], [key], out=ss_ap, in0=ss_ap, scalar1=scale, scalar2=EPS,
               op0=ALU.mult, op1=ALU.add)
            OP("act", "activation", [key], [key], out=ss_ap, in_=ss_ap, func=AF.Sqrt)
            OP("dve", "reciprocal", [key], [key], out=ss_ap, in_=ss_ap)

        with contextlib.ExitStack() as st:
            win_sb = SB(st, "win_sb", [128, 8, INW], BF16)
            wout_sb = SB(st, "wout_sb", [128, 8, D], BF16)
            wa_sb = SB(st, "wa_sb", [128, 4, 128], BF16)
            wx_sb = SB(st, "wx_sb", [128, 4, 128], BF16)
            pw_sb = SB(st, "pw_sb", [128, 4, 128], BF16)
            wr_sb = SB(st, "wr_sb", [128, 8, 36], BF16)
            vec = SB(st, "vec", [128, 56], F32)
            rbias = SB(st, "rbias", [128, 36], F32)
            xt = [SB(st, "xt%d" % i, [128, D], F32) for i in range(8)]
            hn = [SB(st, "hn0", [128, D], BF16)]
            ss1 = SB(st, "ss1", [128, 4], F32)
            hnT = SB(st, "hnT", [128, 8, W], BF16)
            ux = SB(st, "ux", [128, 4, H + W], F32)
            up = SB(st, "up", [128, 4, H + W], F32)
            NS = 2
            xc = [SB(st, "xc%d" % i, [128, W], F32) for i in range(NS)]
            xcb = [SB(st, "xcb%d" % i, [128, W], BF16) for i in range(NS)]
            A4 = SB(st, "A4", [128, 4, W], F32)
            B4 = SB(st, "B4", [128, 4, W], F32)
            C4 = SB(st, "C4", [128, 4, W], F32)
            E4 = SB(st, "E4", [128, 4, W], F32)
            prm = SB(st, "prm", [128, 16], F32)
            y = SB(st, "y", [128, 4, W], F32)
            ysq = SB(st, "ysq", [128, W], BF16)
            yp = SB(st, "yp", [128, 4, W], F32)
            ypsq = SB(st, "ypsq", [128, W], BF16)
            ycat2 = [SB(st, "ycat%d_" % i, [128, 8, W], BF16) for i in range(2)]
            rl = SB(st, "rl", [128, W], F32)
            rp = SB(st, "rp", [128, W], F32)
            P0 = SB(st, "P0", [128, H + W], F32)
            P1 = SB(st, "P1", [128, H + W], F32)
            mb = [SB(st, "mb%d" % i, [128, W], BF16) for i in range(2)]
            hc = SB(st, "hc", [128, 4], F32)
            hn2 = [SB(st, "hn2_%d" % i, [128, D], BF16) for i in range(2)]
            hn2T = [SB(st, "hn2T%d" % i, [128, 8, 128], BF16) for i in range(2)]
            ss2 = SB(st, "ss2", [128, 4], F32)
            lg = SB(st, "lg", [128, 36], F32)
            rt = SB(st, "rt", [128, 16], F32)
            goh = SB(st, "goh", [128, 4], F32)
            pen = SB(st, "pen", [128, 4], F32)
            gs = SB(st, "gs", [128, 48], F32)
            es = SB(st, "es", [128, 32], F32)
            msk = SB(st, "msk", [128, 32], F32)
            M1 = SB(st, "M1", [128, 32], F32)
            M2 = SB(st, "M2", [128, 32], F32)
            Ms = SB(st, "Ms", [128, 32], F32)
            Mb = SB(st, "Mb", [128, 32], BF16)
            Mrun = SB(st, "Mrun", [128, 32], F32)
            Mrunb = SB(st, "Mrunb", [128, 32], BF16)
            posc = SB(st, "posc", [128, 32], F32)
            ovf = SB(st, "ovf", [128, 32], F32)
            tmp32 = SB(st, "tmp32", [128, 32], F32)

            pTT = PS(st, "pTT", [128, 8, 128], BF16)
            pP = [PS(st, "pP0", [128, W], F32)]
            pA = PS(st, "pA", [128, W], F32)
            pX = PS(st, "pX", [128, W], F32)
            pS = PS(st, "pS", [128, W], F32)
            pPp = PS(st, "pPp", [128, W], F32)
            pR = PS(st, "pR", [128, W], F32)
            pO = PS(st, "pO", [128, W], F32)

            DMA("sp", [], ["vec"], out=vec[:], in_=vecs)
            DMA("pool", [], ["win"], out=win_sb[:], in_=w_in.rearrange("(dc p) c -> p dc c", p=128))
            DMA("pool", [], ["wa"], out=wa_sb[:], in_=wa_bd.rearrange("c i j -> i c j"))
            DMA("pool", [], ["wx"], out=wx_sb[:], in_=wx_bd.rearrange("c i j -> i c j"))
            DMA("pool", [], ["pw"], out=pw_sb[:], in_=pool_w.rearrange("g c d -> c g d"))
            DMA("pool", [], ["wout"], out=wout_sb[:], in_=w_out.rearrange("(k p) d -> p k d", p=128))
            DMA("pool", [], ["wr"], out=wr_sb[:], in_=wr.rearrange("(dc p) n -> p dc n", p=128))
            DMA("sp", [], ["rbias"], out=rbias[:], in_=bc[:, 3 * D:3 * D + 36])

            for dc in range(8):
                OP("pool", "tensor_scalar", ["win", "vec"], ["win"], out=win_sb[:, dc, :], in0=win_sb[:, dc, :],
                   scalar1=vec[:, 40 + dc:41 + dc], scalar2=None, op0=ALU.mult)
                OP("dve", "tensor_scalar", ["wr", "vec"], ["wr"], out=wr_sb[:, dc, :], in0=wr_sb[:, dc, :],
                   scalar1=vec[:, 48 + dc:49 + dc], scalar2=None, op0=ALU.mult)
            OP("act", "activation", ["vec"], ["prm"], out=prm[:, 12:16], in_=vec[:, 28:32], func=AF.Sigmoid)
            OP("act", "activation", ["prm"], ["prm"], out=prm[:, 12:16], in_=prm[:, 12:16], func=AF.Ln)
            OP("dve", "tensor_scalar", ["prm"], ["prm"], out=prm[:, 8:12], in0=prm[:, 12:16], scalar1=4.0, scalar2=None,
               op0=ALU.mult)
            OP("dve", "tensor_scalar", ["prm"], ["prm"], out=prm[:, 12:16], in0=prm[:, 12:16], scalar1=8.0, scalar2=None,
               op0=ALU.mult)
            OP("dve", "tensor_scalar", ["prm", "vec"], ["prm"], out=prm[:, 0:8], in0=vec[:, 20:28], scalar1=0.5, scalar2=None,
               op0=ALU.mult)
            OP("pool", "memset", [], ["ux0", "ux1", "ux2", "ux3"], ux[:], 0.0)
            OP("pool", "memset", [], ["up0", "up1", "up2", "up3"], up[:], 0.0)
            OP("pool", "memset", [], ["hc0", "hc1", "hc2", "hc3"], hc[:], 0.0)
            OP("pool", "memset", [], ["Mrun"], Mrun[:], 0.0)
            OP("pool", "memset", [], ["Mrunb"], Mrunb[:], 0.0)

            chunks = [("m", 0, NM)] + [("x", j * W, W) for j in range(NCH)]
            pcount = 0
            lset = 0
            mcount = 0
            tile_idx = 0
            def stageA(ci):
                kind, r0, Wc = chunks[ci]
                ntile = 1 if kind == "m" else 4
                nr = NM if kind == "m" else 128
                xb0 = (ci % 2) * 4
                for tt in range(ntile):
                    b = xb0 + tt
                    src = meta if kind == "m" else x[r0 + tt * 128: r0 + (tt + 1) * 128, :]
                    DMA("sp", [], ["xt%d" % b], out=xt[b][0:nr, :], in_=src)
                    OP("act", "activation", ["xt%d" % b], ["hn0", "ss1"], out=hn[0][0:nr, :], in_=xt[b][0:nr, :],
                       func=AF.Square, accum_out=ss1[0:nr, tt:tt + 1])
                rstd_chain(ss1[0:nr, 0:ntile], "ss1", 1.0 / D)
                for tt in range(ntile):
                    b = xb0 + tt
                    OP("act", "activation", ["xt%d" % b, "ss1"], ["hn0"], out=hn[0][0:nr, :], in_=xt[b][0:nr, :], func=AF.Copy,
                       scale=ss1[0:nr, tt:tt + 1])
                    for hf in range(2):
                        for j in range(4):
                            dc = hf * 4 + j
                            OP("pe", "transpose", ["hn0", "cst_b"], ["pTa"], out=pTT[:, j, 0:nr],
                               in_=hn[0][0:nr, dc * 128:(dc + 1) * 128], identity=ident[0:nr, 0:nr])
                        OP("act", "copy", ["pTa"], ["hnT"], out=hnT[:, hf * 4:hf * 4 + 4, tt * 128: tt * 128 + nr], in_=pTT[:, 0:4, 0:nr])

            def lru(ci):
                nonlocal lset
                kind, r0, Wc = chunks[ci]
                L = H + Wc
                ycat = ycat2[ci % 2]
                yck = "yc%d_" % (ci % 2)

                def proj(oc):
                    for dc in range(8):
                        OP("pe", "matmul", ["win", "hnT"], ["pP0"], pP[0][:, 0:Wc],
                           lhsT=win_sb[:, dc, oc * 128:(oc + 1) * 128], rhs=hnT[:, dc, 0:Wc], start=(dc == 0), stop=(dc == 7))
                    return pP[0], "pP0"

                for cc in range(4):
                    s = lset % NS
                    lset += 1
                    pb, pbk = proj(cc)
                    uk = "ux%d" % cc
                    OP("act", "copy", [pbk], [uk], out=ux[:, cc, H:L], in_=pb[:, 0:Wc])
                    OP("dve", "tensor_scalar", [uk, "vec"], ["xc%d" % s], out=xc[s][:, 0:Wc], in0=ux[:, cc, H - 3:H - 3 + Wc],
                       scalar1=vec[:, cc:cc + 1], scalar2=vec[:, 16 + cc:17 + cc], op0=ALU.mult, op1=ALU.add)
                    for k in range(1, 4):
                        OP("dve", "scalar_tensor_tensor", [uk, "vec", "xc%d" % s], ["xc%d" % s], out=xc[s][:, 0:Wc],
                           in0=ux[:, cc, H - 3 + k:H - 3 + k + Wc], scalar=vec[:, k * 4 + cc:k * 4 + cc + 1],
                           in1=xc[s][:, 0:Wc], op0=ALU.mult, op1=ALU.add)
                    OP("pool", "tensor_copy", [uk], [uk], out=ux[:, cc, 0:H], in_=ux[:, cc, Wc:Wc + H])
                    OP("act", "copy", ["xc%d" % s], ["xcb%d" % s], out=xcb[s][:, 0:Wc], in_=xc[s][:, 0:Wc])
                    OP("pe", "matmul", ["wa", "xcb%d" % s], ["pA"], pA[:, 0:Wc], lhsT=wa_sb[:, cc, :], rhs=xcb[s][:, 0:Wc],
                       start=True, stop=True)
                    OP("pe", "matmul", ["wx", "xcb%d" % s], ["pX"], pX[:, 0:Wc], lhsT=wx_sb[:, cc, :], rhs=xcb[s][:, 0:Wc],
                       start=True, stop=True)
                    OP("act", "activation", ["pA", "prm"], ["A%d" % cc], out=A4[:, cc, 0:Wc], in_=pA[:, 0:Wc], func=AF.Tanh,
                       scale=0.5, bias=prm[:, cc:cc + 1])
                    OP("act", "activation", ["pX", "prm"], ["B%d" % cc], out=B4[:, cc, 0:Wc], in_=pX[:, 0:Wc], func=AF.Tanh,
                       scale=0.5, bias=prm[:, 4 + cc:5 + cc])
                    OP("act", "activation", ["A%d" % cc, "prm"], ["C%d" % cc], out=C4[:, cc, 0:Wc], in_=A4[:, cc, 0:Wc], func=AF.Exp,
                       scale=prm[:, 8 + cc:9 + cc], bias=prm[:, 8 + cc:9 + cc])
                    OP("act", "activation", ["A%d" % cc, "prm"], ["A%d" % cc], out=A4[:, cc, 0:Wc], in_=A4[:, cc, 0:Wc], func=AF.Exp,
                       scale=prm[:, 12 + cc:13 + cc], bias=prm[:, 12 + cc:13 + cc])
                    OP("dve", "scalar_tensor_tensor", ["B%d" % cc, "xc%d" % s], ["B%d" % cc], out=B4[:, cc, 0:Wc], in0=B4[:, cc, 0:Wc],
                       scalar=1.0, in1=xc[s][:, 0:Wc], op0=ALU.add, op1=ALU.mult)
                    pb2, pb2k = proj(4 + cc)
                    OP("act", "copy", [pb2k], ["E%d" % cc], out=E4[:, cc, 0:Wc], in_=pb2[:, 0:Wc])
                Aks = ["A%d" % c for c in range(4)]
                Eks = ["E%d" % c for c in range(4)]
                OP("act", "activation", Aks, Aks, out=A4[:, :, 0:Wc], in_=A4[:, :, 0:Wc], func=AF.Sqrt, scale=-1.0, bias=1.0 + 2.4e-7)
                OP("act", "activation", Eks, Eks, out=E4[:, :, 0:Wc], in_=E4[:, :, 0:Wc], func=AF.Gelu_apprx_tanh)
                for cc in range(4):
                    OP("dve", "scalar_tensor_tensor", ["B%d" % cc, "A%d" % cc], ["B%d" % cc], out=B4[:, cc, 0:Wc], in0=B4[:, cc, 0:Wc],
                       scalar=0.5, in1=A4[:, cc, 0:Wc], op0=ALU.mult, op1=ALU.mult)
                    OP("dve", "tensor_tensor_scan", ["C%d" % cc, "B%d" % cc, "hc%d" % cc], ["y%d" % cc], out=y[:, cc, 0:Wc],
                       data0=C4[:, cc, 0:Wc], data1=B4[:, cc, 0:Wc], initial=hc[:, cc:cc + 1], op0=ALU.mult, op1=ALU.add)
                    OP("act", "copy", ["y%d" % cc], ["hc%d" % cc], out=hc[:, cc:cc + 1], in_=y[:, cc, Wc - 1:Wc])
                    if kind == "m":
                        continue
                    OP("pool", "tensor_tensor", ["y%d" % cc, "E%d" % cc, "hc%d" % cc], ["y%d" % cc], out=y[:, cc, 0:Wc], in0=y[:, cc, 0:Wc],
                       in1=E4[:, cc, 0:Wc], op=ALU.mult)
                    OP("act", "activation", ["y%d" % cc], ["ysq"], out=ysq[:, 0:Wc], in_=y[:, cc, 0:Wc], func=AF.Square)
                    OP("pe", "matmul", ["cst_b", "ysq"], ["pA"], pA[:, 0:Wc], lhsT=ones, rhs=ysq[:, 0:Wc],
                       start=(cc == 0), stop=(cc == 3))
                if kind == "m":
                    return
                OP("act", "activation", ["pA"], ["rl"], out=rl[:, 0:Wc], in_=pA[:, 0:Wc], func=AF.Ln, scale=1.0 / 512, bias=EPS)
                OP("act", "activation", ["rl"], ["rl"], out=rl[:, 0:Wc], in_=rl[:, 0:Wc], func=AF.Exp, scale=-0.5)
                for cc in range(4):
                    OP("dve", "scalar_tensor_tensor", ["y%d" % cc, "vec", "rl"], [yck + "%d" % cc], out=ycat[:, cc, 0:Wc],
                       in0=y[:, cc, 0:Wc], scalar=vec[:, 32 + cc:33 + cc], in1=rl[:, 0:Wc], op0=ALU.mult, op1=ALU.mult)

            def poolbr(ci):
                nonlocal mcount
                kind, r0, Wc = chunks[ci]
                L = H + Wc
                ycat = ycat2[ci % 2]
                yck = "yc%d_" % (ci % 2)
                for g in range(4):
                    for dc in range(8):
                        OP("pe", "matmul", ["win", "hnT"], ["pPp"], pPp[:, 0:Wc],
                           lhsT=win_sb[:, dc, (8 + g) * 128:(9 + g) * 128], rhs=hnT[:, dc, 0:Wc], start=(dc == 0), stop=(dc == 7))
                    uk = "up%d" % g
                    OP("act", "copy", ["pPp"], [uk], out=up[:, g, H:L], in_=pPp[:, 0:Wc])
                    cur, curk, lo = up[:, g, :], uk, 0
                    bufs = [(P0, "P0"), (P1, "P1")]
                    for si, stp in enumerate([1, 2, 4, 8][:g + 1]):
                        dst, dstk = bufs[si % 2]
                        lo2 = lo + stp
                        OP("pool", "tensor_tensor", [curk], [dstk], out=dst[:, lo2:L], in0=cur[:, lo2:L], in1=cur[:, lo:L - stp],
                           op=ALU.add)
                        cur, curk, lo = dst, dstk, lo2
                    wdw = [2, 4, 8, 16][g]
                    mi = mcount % 2
                    mcount += 1
                    if kind == "m":
                        OP("pool", "tensor_copy", [uk], [uk], out=up[:, g, 0:H], in_=up[:, g, Wc:Wc + H])
                        continue
                    OP("dve", "scalar_tensor_tensor", [curk, uk], ["mb%d" % mi], out=mb[mi][:, 0:Wc], in0=cur[:, H:L],
                       scalar=1.0 / wdw, in1=up[:, g, H:L], op0=ALU.mult, op1=ALU.subtract)
                    OP("pool", "tensor_copy", [uk], [uk], out=up[:, g, 0:H], in_=up[:, g, Wc:Wc + H])
                    OP("pe", "matmul", ["pw", "mb%d" % mi], ["pPp"], pPp[:, 0:Wc], lhsT=pw_sb[:, g, :], rhs=mb[mi][:, 0:Wc],
                       start=True, stop=True)
                    OP("act", "copy", ["pPp"], ["yp%d" % g], out=yp[:, g, 0:Wc], in_=pPp[:, 0:Wc])
                    OP("act", "activation", ["pPp"], ["ypsq"], out=ypsq[:, 0:Wc], in_=pPp[:, 0:Wc], func=AF.Square)
                    OP("pe", "matmul", ["cst_b", "ypsq"], ["pS"], pS[:, 0:Wc], lhsT=ones, rhs=ypsq[:, 0:Wc],
                       start=(g == 0), stop=(g == 3))
                if kind == "m":
                    return
                OP("act", "activation", ["pS"], ["rp"], out=rp[:, 0:Wc], in_=pS[:, 0:Wc], func=AF.Ln, scale=1.0 / 512, bias=EPS)
                OP("act", "activation", ["rp"], ["rp"], out=rp[:, 0:Wc], in_=rp[:, 0:Wc], func=AF.Exp, scale=-0.5)
                for g in range(4):
                    OP("dve", "scalar_tensor_tensor", ["yp%d" % g, "vec", "rp"], [yck + "%d" % (4 + g)], out=ycat[:, 4 + g, 0:Wc],
                       in0=yp[:, g, 0:Wc], scalar=vec[:, 36 + g:37 + g], in1=rp[:, 0:Wc], op0=ALU.mult, op1=ALU.mult)

            def tokstage(ci):
                nonlocal tile_idx
                kind, r0, Wc = chunks[ci]
                xb0 = (ci % 2) * 4
                ycat = ycat2[ci % 2]
                yck = "yc%d_" % (ci % 2)
                for tt in range(4):
                    b = xb0 + tt
                    rows = slice(r0 + tt * 128, r0 + (tt + 1) * 128)
                    xk = "xt%d" % b
                    for half in range(2):
                        sl = slice(half * 512, (half + 1) * 512)
                        for k in range(8):
                            OP("pe", "matmul", [yck + "%d" % k, "wout"], ["pO"], pO[:, :],
                               lhsT=ycat[:, k, tt * 128:(tt + 1) * 128], rhs=wout_sb[:, k, sl],
                               start=(k == 0), stop=(k == 7))
                        OP("dve", "tensor_tensor", ["pO", xk], [xk], out=xt[b][:, sl], in0=pO[:, :], in1=xt[b][:, sl],
                           op=ALU.add)
                    DMA("sp", [xk], [], out=h1buf[rows, :], in_=xt[b][:])
                    OP("act", "activation", [xk], ["hn2_%d" % (tt % 2), "ss2"], out=hn2[tt % 2][:], in_=xt[b][:], func=AF.Square,
                       accum_out=ss2[:, tt:tt + 1])
                rstd_chain(ss2[:, 0:4], "ss2", 1.0 / D)
                for tt in range(4):
                    b = xb0 + tt
                    q = tile_idx % 2
                    ti = tile_idx
                    tile_idx += 1
                    xk = "xt%d" % b
                    OP("act", "activation", [xk, "ss2"], ["hn2_%d" % q], out=hn2[q][:], in_=xt[b][:], func=AF.Copy,
                       scale=ss2[:, tt:tt + 1])
                    for hf in range(2):
                        for j in range(4):
                            dc = hf * 4 + j
                            OP("pe", "transpose", ["hn2_%d" % q, "cst_b"], ["pTb"], out=pTT[:, 4 + j, :],
                               in_=hn2[q][:, dc * 128:(dc + 1) * 128], identity=ident)
                        OP("act", "copy", ["pTb"], ["hn2T%d" % q], out=hn2T[q][:, hf * 4:hf * 4 + 4, :], in_=pTT[:, 4:8, :])
                    for dc in range(8):
                        OP("pe", "matmul", ["hn2T%d" % q, "wr"], ["pR"], pR[:, 0:36], lhsT=hn2T[q][:, dc, :], rhs=wr_sb[:, dc, :],
                           start=(dc == 0), stop=(dc == 7))
                    OP("dve", "tensor_tensor", ["pR", "rbias"], ["lg"], out=lg[:], in0=pR[:, 0:36], in1=rbias[:], op=ALU.add)
                    OP("dve", "reduce_max", ["lg"], ["rt0"], out=rt[:, 0:1], in_=lg[:, 0:4], axis=AX.X)
                    OP("dve", "tensor_scalar", ["lg", "rt0"], ["goh"], out=goh[:], in0=lg[:, 0:4], scalar1=rt[:, 0:1], scalar2=None,
                       op0=ALU.is_ge)
                    OP("dve", "tensor_scalar", ["lg", "rt0"], ["gsh"], out=gs[:, 4 * tt:4 * tt + 4], in0=lg[:, 0:4], scalar1=rt[:, 0:1],
                       scalar2=None, op0=ALU.subtract)
                    OP("dve", "tensor_scalar", ["goh"], ["pen"], out=pen[:], in0=goh[:], scalar1=-1.0, scalar2=1.0e30, op0=ALU.add,
                       op1=ALU.mult)
                    for g in range(4):
                        OP("dve", "tensor_scalar", ["lg", "pen"], ["es"], out=es[:, 8 * g:8 * g + 8], in0=lg[:, 4 + 8 * g:12 + 8 * g],
                           scalar1=pen[:, g:g + 1], scalar2=None, op0=ALU.add)
                    OP("dve", "reduce_max", ["es"], ["rt3"], out=rt[:, 3:4], in_=es[:], axis=AX.X)
                    OP("dve", "tensor_scalar", ["es", "rt3"], ["M1"], out=M1[:], in0=es[:], scalar1=rt[:, 3:4], scalar2=None,
                       op0=ALU.is_ge)
                    OP("dve", "scalar_tensor_tensor", ["M1", "es"], ["msk"], out=msk[:], in0=M1[:], scalar=-1e30, in1=es[:],
                       op0=ALU.mult, op1=ALU.add)
                    OP("dve", "reduce_max", ["msk"], ["rt4"], out=rt[:, 4:5], in_=msk[:], axis=AX.X)
                    OP("dve", "tensor_scalar", ["msk", "rt4"], ["M2"], out=M2[:], in0=msk[:], scalar1=rt[:, 4:5], scalar2=None,
                       op0=ALU.is_ge)
                    OP("dve", "tensor_tensor", ["M1", "M2"], ["Ms"], out=Ms[:], in0=M1[:], in1=M2[:], op=ALU.add)
                    OP("dve", "tensor_copy", ["Ms"], ["Mb"], out=Mb[:], in_=Ms[:])
                    OP("pe", "matmul", ["cst_b", "Mb"], ["pR"], pR[:, 64:96], lhsT=tri, rhs=Mb[:], start=True, stop=False)
                    OP("pe", "matmul", ["cst_b", "Mrunb"], ["pR"], pR[:, 64:96], lhsT=ones, rhs=Mrunb[:], start=False, stop=True)
                    OP("dve", "tensor_tensor", ["rt4", "rt3"], ["gdd"], out=gs[:, 32 + tt:33 + tt], in0=rt[:, 4:5], in1=rt[:, 3:4],
                       op=ALU.subtract)
                    OP("dve", "tensor_scalar", ["pR"], ["ovf"], out=ovf[:], in0=pR[:, 64:96], scalar1=float(CAP), scalar2=1.0e6,
                       op0=ALU.is_ge, op1=ALU.mult)
                    OP("dve", "tensor_tensor", ["pR", "cst_f"], ["posc"], out=posc[:], in0=pR[:, 64:96], in1=iotacap, op=ALU.add)
                    OP("dve", "tensor_tensor", ["posc", "ovf"], ["posc"], out=posc[:], in0=posc[:], in1=ovf[:], op=ALU.add)
                    OP("dve", "tensor_tensor", ["Mrun", "Ms"], ["Mrun"], out=Mrun[:], in0=Mrun[:], in1=Ms[:], op=ALU.add)
                    OP("dve", "tensor_copy", ["Mrun"], ["Mrunb"], out=Mrunb[:], in_=Mrun[:])
                    for kk, (Mk, Mkk, Df, Di) in enumerate(((M1, "M1", D0f, D0i), (M2, "M2", D1f, D1i))):
                        OP("dve", "tensor_tensor", [Mkk, "posc"], ["tmp32"], out=tmp32[:], in0=Mk[:], in1=posc[:], op=ALU.mult)
                        OP("dve", "reduce_sum", ["tmp32"], ["Df%d" % kk], out=Df[:, ti:ti + 1], in_=tmp32[:], axis=AX.X)
                        OP("dve", "tensor_copy", ["Df%d" % kk], ["Di%d" % kk], out=Di[ti][:, 0:1], in_=Df[:, ti:ti + 1])
                        SDMA("pool", (lambda e, Di=Di, ti=ti, q=q: e.indirect_dma_start(
                            out=xbuf, out_offset=bass.IndirectOffsetOnAxis(ap=Di[ti][:, 0:1], axis=0),
                            in_=hn2[q][:], in_offset=None, bounds_check=S.bcreg, oob_is_err=False)),
                            ["hn2_%d" % q, "Di%d" % kk] + xzkeys, [])
                ti0 = tile_idx - 4
                OP("act", "activation", ["gsh"], ["gex"], out=gs[:, 16:32], in_=gs[:, 0:16], func=AF.Exp)
                OP("dve", "reduce_sum", ["gex"], ["gse"], out=gs[:, 36:40], in_=gs[:, 16:32].rearrange("p (t g) -> p t g", g=4), axis=AX.X)
                OP("act", "activation", ["gdd"], ["ged"], out=gs[:, 40:44], in_=gs[:, 32:36], func=AF.Exp)
                OP("dve", "scalar_tensor_tensor", ["ged", "gse"], ["gt4"], out=gs[:, 44:48], in0=gs[:, 40:44], scalar=1.0, in1=gs[:, 36:40],
                   op0=ALU.add, op1=ALU.mult)
                OP("dve", "reciprocal", ["gt4"], ["G0"], out=G0[:, ti0:ti0 + 4], in_=gs[:, 44:48])
                OP("dve", "tensor_tensor", ["G0", "ged"], ["G1"], out=G1[:, ti0:ti0 + 4], in0=G0[:, ti0:ti0 + 4], in1=gs[:, 40:44],
                   op=ALU.mult)
            yc0keys = ["yc0_%d" % k for k in range(8)]
            NZ = NE * CAP // 512
            xzkeys = ["xz%d" % i for i in range(NZ)]

            def zero_fill():
                OP("pool", "memset", [], yc0keys, ycat2[0][:], 0.0)
                for i in range(NZ):
                    DMA("pool", yc0keys, ["xz%d" % i],
                        out=xbuf[i * 512:(i + 1) * 512, :].rearrange("(p r) d -> p (r d)", r=4),
                        in_=ycat2[0][:].rearrange("p k w -> p (k w)"))

            commit(record(stageA, 0))
            commit(record(lru, 0), record(poolbr, 0))
            commit(record(stageA, 1))
            lz = [(k_, q_, f_, r_, w_, 4.0) for (k_, q_, f_, r_, w_, c_) in record(zero_fill)]
            commit(record(lru, 1), record(poolbr, 1), lz)
            for ci in range(1, len(chunks)):
                la = record(tokstage, ci)
                if ci + 1 < len(chunks):
                    lA = record(stageA, ci + 1)
                    ll = record(lru, ci + 1)
                    lp = record(poolbr, ci + 1)
                    n1 = (len(la) * len(lA)) // (len(lA) + max(len(ll), len(lp)))
                    commit(la[:n1], lA)
                    commit(la[n1:], ll, lp)
                else:
                    commit(la)
            if debug:
                print("phase1 sbuf remaining", nc.sbuf_bytes_remaining)
                DMA("sp", ["G0"], [], out=dbg_route[:, 0:NT], in_=G0[:])
                DMA("sp", ["G1"], [], out=dbg_route[:, NT:2 * NT], in_=G1[:])
                DMA("sp", ["Df0"], [], out=dbg_route[:, 2 * NT:3 * NT], in_=D0f[:])
                DMA("sp", ["Df1"], [], out=dbg_route[:, 3 * NT:4 * NT], in_=D1f[:])
            S.emit()

        S.barrier()
        with contextlib.ExitStack() as st:
            NWB = 3
            wg_sb = [SB(st, "wg_sb%d" % i, [128, 8, FF], BF16) for i in range(NWB)]
            wu_sb = [SB(st, "wu_sb%d" % i, [128, 8, FF], BF16) for i in range(NWB)]
            wd_sb = [SB(st, "wd_sb%d" % i, [128, 4, D], BF16) for i in range(NWB)]
            xe = [SB(st, "xe%d" % i, [128, NB, D], BF16) for i in range(NWB)]
            xeT = [SB(st, "xeT%d" % i, [128, 8, CAP], BF16) for i in range(2)]
            hidT = [SB(st, "hidT%d" % i, [128, 4, CAP], BF16) for i in range(2)]
            sg = [SB(st, "sg%d" % i, [128, CAP], F32) for i in range(2)]
            ot = [SB(st, "ot%d" % i, [128, D], BF16) for i in range(3)]
            g2bc = SB(st, "g2bc_sb", [128, 8, 128], F32)
            DMA("sp", [], ["g2bc"], out=g2bc[:], in_=g2bc_d.rearrange("p (k j) -> p k j", j=128))
            pT2 = [PS(st, "pT2_%d" % i, [128, 8, 128], BF16) for i in range(2)]
            pG = [PS(st, "pG%d" % i, [128, 512], F32) for i in range(2)]
            pU = [PS(st, "pU%d" % i, [128, 512], F32) for i in range(2)]
            pO2 = PS(st, "pO2", [128, D], F32)

            def loads(e):
                wb = e % NWB
                DMA("pool", [], ["wg%d" % wb], out=wg_sb[wb][:], in_=wgate[e].rearrange("(dc p) f -> p dc f", p=128))
                DMA("pool", [], ["wu%d" % wb], out=wu_sb[wb][:], in_=wup[e].rearrange("(dc p) f -> p dc f", p=128))
                DMA("pool", [], ["wd%d" % wb], out=wd_sb[wb][:], in_=wdown[e].rearrange("(fc p) d -> p fc d", p=128))
                DMA("sp", [], ["xe%d" % wb], out=xe[wb][:], in_=xbuf[e * CAP:(e + 1) * CAP, :].rearrange("(b p) d -> p b d", p=128))

            tcnt = 0
            ocnt = 0

            def front(e):
                nonlocal tcnt
                wb = e % NWB
                xb2 = e % 2
                for bb in range(NB):
                    ti2 = tcnt % 2
                    tcnt += 1
                    for dc in range(8):
                        OP("pe", "transpose", ["xe%d" % wb, "cst_b"], ["pT2_%d" % ti2], out=pT2[ti2][:, dc, :],
                           in_=xe[wb][:, bb, dc * 128:(dc + 1) * 128], identity=ident)
                    OP("dve", "tensor_tensor", ["pT2_%d" % ti2, "g2bc"], ["xeT%d" % xb2], out=xeT[xb2][:, :, bb * 128:(bb + 1) * 128],
                       in0=pT2[ti2][:], in1=g2bc[:], op=ALU.mult)
                for fc in range(4):
                    pi = fc % 2
                    for dc in range(8):
                        OP("pe", "matmul", ["wg%d" % wb, "xeT%d" % xb2], ["pG%d" % pi], pG[pi][:, 0:CAP],
                           lhsT=wg_sb[wb][:, dc, fc * 128:(fc + 1) * 128], rhs=xeT[xb2][:, dc, :], start=(dc == 0), stop=(dc == 7))
                    for dc in range(8):
                        OP("pe", "matmul", ["wu%d" % wb, "xeT%d" % xb2], ["pU%d" % pi], pU[pi][:, 0:CAP],
                           lhsT=wu_sb[wb][:, dc, fc * 128:(fc + 1) * 128], rhs=xeT[xb2][:, dc, :], start=(dc == 0), stop=(dc == 7))
                    OP("act", "activation", ["pG%d" % pi], ["sg%d" % pi], out=sg[pi][:], in_=pG[pi][:, 0:CAP], func=AF.Silu)
                    OP("dve", "tensor_tensor", ["sg%d" % pi, "pU%d" % pi], ["hid%d_%d" % (xb2, fc)], out=hidT[xb2][:, fc, :],
                       in0=sg[pi][:], in1=pU[pi][:, 0:CAP], op=ALU.mult)

            def back(e):
                nonlocal ocnt
                wb = e % NWB
                xb2 = e % 2
                for bb in range(NB):
                    oi = ocnt % 3
                    ocnt += 1
                    for half in range(2):
                        for fc in range(4):
                            OP("pe", "matmul", ["hid%d_%d" % (xb2, fc), "wd%d" % wb], ["pO2_%d" % half],
                               pO2[:, half * 512:(half + 1) * 512], lhsT=hidT[xb2][:, fc, bb * 128:(bb + 1) * 128],
                               rhs=wd_sb[wb][:, fc, half * 512:(half + 1) * 512], start=(fc == 0), stop=(fc == 3))
                    OP("act", "copy", ["pO2_0"], ["ot%d" % oi], out=ot[oi][:, 0:512], in_=pO2[:, 0:512])
                    OP("dve", "tensor_copy", ["pO2_1"], ["ot%db" % oi], out=ot[oi][:, 512:1024], in_=pO2[:, 512:1024])
                    r1 = e * CAP + bb * 128
                    DMA("sp", ["ot%d" % oi, "ot%db" % oi], [], out=obuf[r1:r1 + 128, :], in_=ot[oi][:])

            loads(0)
            loads(1)
            commit(record(front, 0))
            for e in range(NE):
                ls = [record(back, e)]
                if e + 1 < NE:
                    ls.append(record(front, e + 1))
                if e + 2 < NE:
                    ls.append(record(loads, e + 2))
                commit(*ls)
            S.emit()

        S.barrier()
        with contextlib.ExitStack() as st:
            gfb = SB(st, "gfb", [128, D], F32)
            NB3 = 4
            h1t = [SB(st, "h1t%d" % i, [128, D], F32) for i in range(NB3)]
            o0 = [SB(st, "o0_%d" % i, [128, D], BF16) for i in range(NB3)]
            o1 = [SB(st, "o1_%d" % i, [128, D], BF16) for i in range(NB3)]
            outt = [SB(st, "outt%d" % i, [128, D], F32) for i in range(NB3)]
            junk = SB(st, "junk", [128, D], BF16)
            ssf = [SB(st, "ssf%d" % i, [128, 1], F32) for i in range(NB3)]
            DMA("sp", [], ["gfb"], out=gfb[:], in_=bc[:, 2 * D:3 * D])
            def p3_fetch(ti):
                b = ti % NB3
                rows = slice(ti * 128, (ti + 1) * 128)
                DMA("sp", [], ["h1t%d" % b], out=h1t[b][:], in_=h1buf[rows, :])
                OP("act", "memzero", [], ["o0_%d" % b], o0[b][:])
                OP("act", "memzero", [], ["o1_%d" % b], o1[b][:])
                for (ob, okey, Di) in ((o0, "o0_%d" % b, D0i), (o1, "o1_%d" % b, D1i)):
                    SDMA("pool", (lambda e, ob=ob, Di=Di, ti=ti, b=b: e.indirect_dma_start(
                        out=ob[b][:], out_offset=None, in_=obuf,
                        in_offset=bass.IndirectOffsetOnAxis(ap=Di[ti][:, 0:1], axis=0),
                        bounds_check=S.bcreg, oob_is_err=False)), [okey], [okey])

            def p3_compute(ti):
                b = ti % NB3
                rows = slice(ti * 128, (ti + 1) * 128)
                OP("dve", "scalar_tensor_tensor", ["o0_%d" % b, "h1t%d" % b], ["h1t%d" % b], out=h1t[b][:], in0=o0[b][:],
                   scalar=G0[:, ti:ti + 1], in1=h1t[b][:], op0=ALU.mult, op1=ALU.add)
                OP("dve", "scalar_tensor_tensor", ["o1_%d" % b, "h1t%d" % b], ["h1t%d" % b], out=h1t[b][:], in0=o1[b][:],
                   scalar=G1[:, ti:ti + 1], in1=h1t[b][:], op0=ALU.mult, op1=ALU.add)
                sk = "ssf%d" % b
                OP("act", "activation", ["h1t%d" % b], ["junk", sk], out=junk[:], in_=h1t[b][:], func=AF.Square,
                   accum_out=ssf[b][:, 0:1])
                rstd_chain(ssf[b][:, 0:1], sk, 1.0 / D)
                OP("dve", "scalar_tensor_tensor", ["h1t%d" % b, sk, "gfb"], ["outt%d" % b], out=outt[b][:], in0=h1t[b][:],
                   scalar=ssf[b][:, 0:1], in1=gfb[:], op0=ALU.mult, op1=ALU.mult)
                DMA("sp", ["outt%d" % b], [], out=out[rows, :], in_=outt[b][:])

            p3_fetch(0)
            p3_fetch(1)
            for ti in range(NT):
                if ti + 2 < NT:
                    p3_fetch(ti + 2)
                p3_compute(ti)
            S.emit(final=True)
    return nc


def _host_layout(inputs):
    f = lambda k: np.asarray(inputs[k], dtype=np.float32)
    conv_w = f("conv_w")[0]
    vecs = np.zeros((128, 56), np.float32)
    for k in range(4):
        vecs[:, k * 4:(k + 1) * 4] = conv_w[k].reshape(4, 128).T
    for i, name in enumerate(["conv_b", "lru_ba", "lru_bx", "lru_lambda", "lru_out_gain", "pool_scale"]):
        vecs[:, 16 + 4 * i:20 + 4 * i] = f(name)[0].reshape(4, 128).T
    vecs[:, 40:48] = f("norm1_gain")[0].reshape(8, 128).T
    vecs[:, 48:56] = f("norm2_gain")[0].reshape(8, 128).T
    g2bc = np.ascontiguousarray(np.repeat(vecs[:, 48:56], 128, axis=1))
    rb = np.concatenate([f("b_group")[0], f("b_router")[0]])
    row = np.concatenate([f("norm1_gain")[0], f("norm2_gain")[0], f("final_gain"), rb])
    bc = np.ascontiguousarray(np.broadcast_to(row[None, :], (128, row.shape[0]))).astype(np.float32)

    def blockdiag(w):
        o = np.zeros((4, 128, 128), np.float32)
        for h in range(8):
            c, r = h // 2, (h % 2) * 64
            o[c, r:r + 64, r:r + 64] = w[h]
        return o

    wa_bd = blockdiag(f("lru_wa")[0])
    wx_bd = blockdiag(f("lru_wx")[0])
    wr = np.ascontiguousarray(np.concatenate([f("w_group")[0], f("w_router")[0]], axis=1))
    cst = np.zeros((128, 480), np.float32)
    cst[:, 0:128] = np.eye(128, dtype=np.float32)
    cst[:, 128:256] = np.triu(np.ones((128, 128), np.float32), k=1)
    cst[:, 256:384] = 1.0
    cst[:, 384:416] = (np.arange(32, dtype=np.float32) * CAP)[None, :]
    for g, w in enumerate((2, 4, 8, 16)):
        cst[:, 416 + g * 16:416 + (g + 1) * 16] = (1.0 / np.minimum(np.arange(1, 17), w)).astype(np.float32)[None, :]
    shared = {
        "meta": np.ascontiguousarray(f("meta_tokens")),
        "w_in": np.ascontiguousarray(f("w_in")[0]),
        "w_out": np.ascontiguousarray(f("w_out")[0]),
        "w_gate": np.ascontiguousarray(f("w_gate")[0]),
        "w_up": np.ascontiguousarray(f("w_up")[0]),
        "w_down": np.ascontiguousarray(f("w_down")[0]),
        "vecs": vecs, "g2bc": g2bc, "bc": bc, "wa_bd": wa_bd, "wx_bd": wx_bd,
        "pool_w": np.ascontiguousarray(f("pool_w")[0]), "wr": wr, "cst": cst,
    }
    return shared


def kernel(**inputs):
    shared = _host_layout(inputs)
    x = np.asarray(inputs["x"], dtype=np.float32)
    nc = build()
    in_maps = []
    for c in range(NCORES):
        m = dict(shared)
        m["x"] = np.ascontiguousarray(x[c])
        in_maps.append(m)
    res = run_bass_kernel_spmd(nc, in_maps, core_ids=list(range(NCORES)))
    return np.stack([np.asarray(r["out"], dtype=np.float32) for r in res.results], axis=0)
```

```python
import contextlib
import numpy as np
import concourse.bass as bass
import concourse.mybir as mybir
from concourse.bass_utils import run_bass_kernel_spmd

F32 = mybir.dt.float32
BF16 = mybir.dt.bfloat16
I32 = mybir.dt.int32
AF = mybir.ActivationFunctionType
ALU = mybir.AluOpType
AX = mybir.AxisListType

D = 1024
NX = 4096
NM = 16
W = 512
H = 16
NCH = NX // W
NE = 32
CAP = 512
NB = CAP // 128
FF = 512
INW = 1536
EPS = 1e-6
NT = NX // 128
NCORES = 8
GRAN = 1


class Sched:
    ENGS = ("pe", "act", "dve", "pool", "sp")

    def __init__(self, nc, stack, n_dma_sems=64):
        self.nc = nc
        self.ops = {e: [] for e in self.ENGS}
        self.seq = {e: 0 for e in self.ENGS}
        self.waited = {e: {} for e in self.ENGS}
        self.last_w = {}
        self.readers = {}
        self.esem = {e: stack.enter_context(nc.semaphore("s_" + e)) for e in self.ENGS if e != "sp"}
        self.dsem = [stack.enter_context(nc.semaphore("d_%d" % i)) for i in range(n_dma_sems)]
        self.duse = [0] * n_dma_sems
        half = n_dma_sems // 2
        self.ring = {"sw": list(range(0, half)), "hw": list(range(half, n_dma_sems))}
        self.rnext = {"sw": 0, "hw": 0}
        self.barrier_tokens = []

    def _sem(self, key):
        return self.esem[key] if isinstance(key, str) else self.dsem[key]

    def _deps(self, eng, reads, writes, extra=()):
        deps = list(extra) + self.barrier_tokens
        for r in reads:
            t = self.last_w.get(r)
            if t is not None:
                deps.append(t)
        for w in writes:
            t = self.last_w.get(w)
            if t is not None:
                deps.append(t)
            deps.extend(self.readers.get(w, ()))
        waits = {}
        for (k, v) in deps:
            if eng == "pe" and k == "pe":
                continue
            if self.waited[eng].get(k, 0) >= v:
                continue
            if waits.get(k, 0) < v:
                waits[k] = v
        for k, v in waits.items():
            self.waited[eng][k] = v
        return list(waits.items())

    def _commit(self, tok, reads, writes):
        for r in reads:
            self.readers.setdefault(r, []).append(tok)
        for w in writes:
            self.last_w[w] = tok
            self.readers[w] = []

    def op(self, eng, fn, reads=(), writes=()):
        waits = self._deps(eng, reads, writes)
        self.seq[eng] += 1
        tok = (eng, self.seq[eng])
        self.ops[eng].append((waits, fn, self.esem[eng], 1))
        self._commit(tok, reads, writes)
        return tok

    def dma(self, q, fn, reads=(), writes=()):
        rk = "sw" if q == "pool" else "hw"
        j = self.ring[rk][self.rnext[rk]]
        self.rnext[rk] = (self.rnext[rk] + 1) % len(self.ring[rk])
        extra = []
        if self.duse[j] > 0:
            extra.append((j, 16 * self.duse[j]))
        waits = self._deps(q, reads, writes, extra)
        self.duse[j] += 1
        tok = (j, 16 * self.duse[j])
        self.ops[q].append((waits, fn, self.dsem[j], 16))
        self._commit(tok, reads, writes)
        return tok

    def all_tokens(self):
        toks = [(j, 16 * u) for j, u in enumerate(self.duse) if u > 0]
        toks += [(e, self.seq[e]) for e in self.ENGS if e != "sp" and self.seq[e] > 0]
        return toks

    def barrier(self):
        self.barrier_tokens = self.all_tokens()

    def emit(self, final=False):
        nc = self.nc
        fin = []
        if final:
            for (k, v) in self.all_tokens():
                if self.waited["sp"].get(k, 0) < v:
                    fin.append((k, v))
        sched = self
        ops = self.ops
        self.ops = {e: [] for e in self.ENGS}

        def run(engname, eng):
            if engname == "pool":
                sched.bcreg = eng.to_reg(NE * CAP - 1)
            for (waits, fn, sem, inc) in ops[engname]:
                for (k, v) in waits:
                    eng.wait_ge(sched._sem(k), v)
                fn(eng).then_inc(sem, inc)
            if engname == "sp":
                for (k, v) in fin:
                    eng.wait_ge(sched._sem(k), v)

        with nc.Block() as block:
            @block.tensor
            def _(eng):
                run("pe", eng)

            @block.scalar
            def _(eng):
                run("act", eng)

            @block.vector
            def _(eng):
                run("dve", eng)

            @block.gpsimd
            def _(eng):
                run("pool", eng)

            @block.sync
            def _(eng):
                run("sp", eng)


def build(debug=False):
    nc = bass.Bass("TRN2", target_bir_lowering=False)

    def din(name, shape, dtype=F32):
        return nc.dram_tensor(name, shape, dtype, kind="ExternalInput").ap()

    x = din("x", [NX, D])
    meta = din("meta", [NM, D])
    w_in = din("w_in", [D, INW])
    w_out = din("w_out", [D, D])
    wgate = din("w_gate", [NE, D, FF])
    wup = din("w_up", [NE, D, FF])
    wdown = din("w_down", [NE, FF, D])
    vecs = din("vecs", [128, 40])
    bc = din("bc", [128, 3 * D + 36])
    wa_bd = din("wa_bd", [4, 128, 128])
    wx_bd = din("wx_bd", [4, 128, 128])
    pool_w = din("pool_w", [4, 128, 128])
    wr = din("wr", [D, 36])
    cst = din("cst", [128, 480])
    out = nc.dram_tensor("out", [NX, D], F32, kind="ExternalOutput").ap()
    dk = "ExternalOutput" if debug else "Internal"
    h1buf = nc.dram_tensor("h1buf", [NX, D], F32, kind=dk).ap()
    xbuf = nc.dram_tensor("xbuf", [NE * CAP, D], BF16, kind=dk).ap()
    obuf = nc.dram_tensor("obuf", [NE * CAP, D], BF16, kind=dk).ap()
    if debug:
        dbg_route = nc.dram_tensor("dbg_route", [128, 4 * NT], F32, kind="ExternalOutput").ap()

    with contextlib.ExitStack() as gst:
        S = Sched(nc, gst)

        defer = {"list": None}

        def est_cost(eng, method, n):
            if eng == "pe":
                return max(64, n) / 2400.0 + 0.02
            if eng == "act":
                return 0.22 + n / 1150.0
            if eng == "dve":
                f = {"scalar_tensor_tensor": 1.6, "tensor_tensor_scan": 2.4, "reciprocal": 6.0}.get(method, 1.0)
                return 0.15 + f * n / 960.0
            return 0.25 + n / 500.0

        def OP(eng, method, reads, writes, *args, **kw):
            fn = lambda e: getattr(e, method)(*args, **kw)
            if defer["list"] is not None:
                o = kw.get("out", args[0] if args else None)
                n = 1
                for d in list(o.shape)[1:]:
                    n *= int(d)
                defer["list"].append(("op", eng, fn, list(reads), list(writes), est_cost(eng, method, n)))
            else:
                S.op(eng, fn, reads, writes)

        def SDMA(q, fn, reads, writes):
            if defer["list"] is not None:
                defer["list"].append(("dma", q, fn, list(reads), list(writes), 1.1 if q == "pool" else 0.1))
            else:
                S.dma(q, fn, reads, writes)

        def DMA(q, reads, writes, **kw):
            SDMA(q, lambda e: e.dma_start(**kw), reads, writes)

        def record(f, *a):
            defer["list"] = []
            f(*a)
            out_l = defer["list"]
            defer["list"] = None
            return out_l

        sim = {"free": {e: 0.0 for e in Sched.ENGS}, "wfin": {}, "rfin": {}}

        def commit(*lists):
            lists = [l for l in lists if l]
            pos = [0] * len(lists)
            total = sum(len(l) for l in lists)
            free, wfin, rfin = sim["free"], sim["wfin"], sim["rfin"]
            for _ in range(total):
                best = None
                for k, l in enumerate(lists):
                    if pos[k] >= len(l):
                        continue
                    kind, q, fn, reads, writes, cost = l[pos[k]]
                    ready = 0.0
                    for r in reads:
                        ready = max(ready, wfin.get(r, 0.0))
                    for w in writes:
                        ready = max(ready, wfin.get(w, 0.0), rfin.get(w, 0.0))
                    start = max(free[q], ready + 0.05)
                    cand = (start, pos[k] / len(l), k)
                    if best is None or cand < best:
                        best = cand
                start, _, k = best
                kind, q, fn, reads, writes, cost = lists[k][pos[k]]
                pos[k] += 1
                free[q] = start + cost
                fin = start + cost + (3.0 if kind == "dma" else 0.0)
                for r in reads:
                    rfin[r] = max(rfin.get(r, 0.0), fin)
                for w in writes:
                    wfin[w] = fin
                    rfin[w] = 0.0
                if kind == "op":
                    S.op(q, fn, reads, writes)
                else:
                    S.dma(q, fn, reads, writes)

        def SB(stack, name, shape, dtype):
            return stack.enter_context(nc.sbuf_tensor(name, shape, dtype))

        def PS(stack, name, shape, dtype):
            return stack.enter_context(nc.psum_tensor(name, shape, dtype))

        cst_b = SB(gst, "cst_b", [128, 384], BF16)
        cst_f = SB(gst, "cst_f", [128, 96], F32)
        D0f = SB(gst, "D0f", [128, NT], F32)
        D1f = SB(gst, "D1f", [128, NT], F32)
        D0i = [SB(gst, "D0i%d" % i, [128, 1], I32) for i in range(NT)]
        D1i = [SB(gst, "D1i%d" % i, [128, 1], I32) for i in range(NT)]
        G0 = SB(gst, "G0", [128, NT], F32)
        G1 = SB(gst, "G1", [128, NT], F32)
        ident = cst_b[:, 0:128]
        tri = cst_b[:, 128:256]
        ones = cst_b[:, 256:384]
        iotacap = cst_f[:, 0:32]
        rec = cst_f[:, 32:96]

        DMA("pool", [], ["cst_b"], out=cst_b[:], in_=cst[:, 0:384])
        DMA("sp", [], ["cst_f"], out=cst_f[:], in_=cst[:, 384:480])

        def rstd_chain(ss_ap, key, scale):
            OP("dve", "tensor_scalar", [key], [key], out=ss_ap, in0=ss_ap, scalar1=scale, scalar2=EPS,
               op0=ALU.mult, op1=ALU.add)
            OP("act", "activation", [key], [key], out=ss_ap, in_=ss_ap, func=AF.Sqrt)
            OP("dve", "reciprocal", [key], [key], out=ss_ap, in_=ss_ap)

        with contextlib.ExitStack() as st:
            win_sb = SB(st, "win_sb", [128, 8, INW], BF16)
            wout_sb = SB(st, "wout_sb", [128, 8, D], BF16)
            wa_sb = SB(st, "wa_sb", [128, 4, 128], BF16)
            wx_sb = SB(st, "wx_sb", [128, 4, 128], BF16)
            pw_sb = SB(st, "pw_sb", [128, 4, 128], BF16)
            wr_sb = SB(st, "wr_sb", [128, 8, 36], BF16)
            vec = SB(st, "vec", [128, 40], F32)
            g1b = SB(st, "g1b", [128, D], F32)
            g2b = SB(st, "g2b", [128, D], F32)
            rbias = SB(st, "rbias", [128, 36], F32)
            xt = [SB(st, "xt%d" % i, [128, D], F32) for i in range(8)]
            hn = [SB(st, "hn0", [128, D], BF16)]
            ss1 = SB(st, "ss1", [128, 4], F32)
            hnT = SB(st, "hnT", [128, 8, W], BF16)
            ux = SB(st, "ux", [128, 4, H + W], F32)
            up = SB(st, "up", [128, 4, H + W], F32)
            NS = 2
            xc = [SB(st, "xc%d" % i, [128, W], F32) for i in range(NS)]
            xcb = [SB(st, "xcb%d" % i, [128, W], BF16) for i in range(NS)]
            A4 = SB(st, "A4", [128, 4, W], F32)
            B4 = SB(st, "B4", [128, 4, W], F32)
            C4 = SB(st, "C4", [128, 4, W], F32)
            E4 = SB(st, "E4", [128, 4, W], F32)
            prm = SB(st, "prm", [128, 16], F32)
            y = SB(st, "y", [128, 4, W], F32)
            ysq = SB(st, "ysq", [128, W], BF16)
            yp = SB(st, "yp", [128, 4, W], F32)
            ypsq = SB(st, "ypsq", [128, W], BF16)
            ycat2 = [SB(st, "ycat%d_" % i, [128, 8, W], BF16) for i in range(2)]
            rl = SB(st, "rl", [128, W], F32)
            rp = SB(st, "rp", [128, W], F32)
            P0 = SB(st, "P0", [128, H + W], F32)
            P1 = SB(st, "P1", [128, H + W], F32)
            mb = [SB(st, "mb%d" % i, [128, W], BF16) for i in range(2)]
            hc = SB(st, "hc", [128, 4], F32)
            hn2 = [SB(st, "hn2_%d" % i, [128, D], BF16) for i in range(2)]
            hn2T = [SB(st, "hn2T%d" % i, [128, 8, 128], BF16) for i in range(2)]
            ss2 = SB(st, "ss2", [128, 4], F32)
            lg = SB(st, "lg", [128, 36], F32)
            rt = SB(st, "rt", [128, 16], F32)
            goh = SB(st, "goh", [128, 4], F32)
            pen = SB(st, "pen", [128, 4], F32)
            gs = SB(st, "gs", [128, 48], F32)
            es = SB(st, "es", [128, 32], F32)
            msk = SB(st, "msk", [128, 32], F32)
            M1 = SB(st, "M1", [128, 32], F32)
            M2 = SB(st, "M2", [128, 32], F32)
            Ms = SB(st, "Ms", [128, 32], F32)
            Mb = SB(st, "Mb", [128, 32], BF16)
            Mrun = SB(st, "Mrun", [128, 32], F32)
            Mrunb = SB(st, "Mrunb", [128, 32], BF16)
            posc = SB(st, "posc", [128, 32], F32)
            ovf = SB(st, "ovf", [128, 32], F32)
            tmp32 = SB(st, "tmp32", [128, 32], F32)

            pTT = PS(st, "pTT", [128, 8, 128], BF16)
            pP = [PS(st, "pP0", [128, W], F32)]
            pA = PS(st, "pA", [128, W], F32)
            pX = PS(st, "pX", [128, W], F32)
            pS = PS(st, "pS", [128, W], F32)
            pPp = PS(st, "pPp", [128, W], F32)
            pR = PS(st, "pR", [128, W], F32)
            pO = PS(st, "pO", [128, W], F32)

            DMA("sp", [], ["vec"], out=vec[:], in_=vecs)
            DMA("sp", [], ["g1b"], out=g1b[:], in_=bc[:, 0:D])
            DMA("pool", [], ["win"], out=win_sb[:], in_=w_in.rearrange("(dc p) c -> p dc c", p=128))
            DMA("pool", [], ["wa"], out=wa_sb[:], in_=wa_bd.rearrange("c i j -> i c j"))
            DMA("pool", [], ["wx"], out=wx_sb[:], in_=wx_bd.rearrange("c i j -> i c j"))
            DMA("pool", [], ["pw"], out=pw_sb[:], in_=pool_w.rearrange("g c d -> c g d"))
            DMA("pool", [], ["wout"], out=wout_sb[:], in_=w_out.rearrange("(k p) d -> p k d", p=128))
            DMA("pool", [], ["wr"], out=wr_sb[:], in_=wr.rearrange("(dc p) n -> p dc n", p=128))
            DMA("sp", [], ["g2b"], out=g2b[:], in_=bc[:, D:2 * D])
            DMA("sp", [], ["rbias"], out=rbias[:], in_=bc[:, 3 * D:3 * D + 36])

            OP("act", "activation", ["vec"], ["prm"], out=prm[:, 12:16], in_=vec[:, 28:32], func=AF.Sigmoid)
            OP("act", "activation", ["prm"], ["prm"], out=prm[:, 12:16], in_=prm[:, 12:16], func=AF.Ln)
            OP("dve", "tensor_scalar", ["prm"], ["prm"], out=prm[:, 8:12], in0=prm[:, 12:16], scalar1=4.0, scalar2=None,
               op0=ALU.mult)
            OP("dve", "tensor_scalar", ["prm"], ["prm"], out=prm[:, 12:16], in0=prm[:, 12:16], scalar1=8.0, scalar2=None,
               op0=ALU.mult)
            OP("dve", "tensor_scalar", ["prm", "vec"], ["prm"], out=prm[:, 0:8], in0=vec[:, 20:28], scalar1=0.5, scalar2=None,
               op0=ALU.mult)
            OP("pool", "memset", [], ["ux0", "ux1", "ux2", "ux3"], ux[:], 0.0)
            OP("pool", "memset", [], ["up0", "up1", "up2", "up3"], up[:], 0.0)
            OP("pool", "memset", [], ["hc0", "hc1", "hc2", "hc3"], hc[:], 0.0)
            OP("pool", "memset", [], ["Mrun"], Mrun[:], 0.0)
            OP("pool", "memset", [], ["Mrunb"], Mrunb[:], 0.0)

            chunks = [("m", 0, NM)] + [("x", j * W, W) for j in range(NCH)]
            pcount = 0
            lset = 0
            mcount = 0
            tile_idx = 0
            def stageA(ci):
                kind, r0, Wc = chunks[ci]
                ntile = 1 if kind == "m" else 4
                nr = NM if kind == "m" else 128
                xb0 = (ci % 2) * 4
                for tt in range(ntile):
                    b = xb0 + tt
                    src = meta if kind == "m" else x[r0 + tt * 128: r0 + (tt + 1) * 128, :]
                    DMA("sp", [], ["xt%d" % b], out=xt[b][0:nr, :], in_=src)
                    OP("act", "activation", ["xt%d" % b], ["hn0", "ss1"], out=hn[0][0:nr, :], in_=xt[b][0:nr, :],
                       func=AF.Square, accum_out=ss1[0:nr, tt:tt + 1])
                rstd_chain(ss1[0:nr, 0:ntile], "ss1", 1.0 / D)
                for tt in range(ntile):
                    b = xb0 + tt
                    OP("dve", "scalar_tensor_tensor", ["xt%d" % b, "ss1", "g1b"], ["hn0"], out=hn[0][0:nr, :],
                       in0=xt[b][0:nr, :], scalar=ss1[0:nr, tt:tt + 1], in1=g1b[0:nr, :], op0=ALU.mult, op1=ALU.mult)
                    for hf in range(2):
                        for j in range(4):
                            dc = hf * 4 + j
                            OP("pe", "transpose", ["hn0", "cst_b"], ["pTa"], out=pTT[:, j, 0:nr],
                               in_=hn[0][0:nr, dc * 128:(dc + 1) * 128], identity=ident[0:nr, 0:nr])
                        OP("act", "copy", ["pTa"], ["hnT"], out=hnT[:, hf * 4:hf * 4 + 4, tt * 128: tt * 128 + nr], in_=pTT[:, 0:4, 0:nr])

            def lru(ci):
                nonlocal lset
                kind, r0, Wc = chunks[ci]
                L = H + Wc
                ycat = ycat2[ci % 2]
                yck = "yc%d_" % (ci % 2)

                def proj(oc):
                    for dc in range(8):
                        OP("pe", "matmul", ["win", "hnT"], ["pP0"], pP[0][:, 0:Wc],
                           lhsT=win_sb[:, dc, oc * 128:(oc + 1) * 128], rhs=hnT[:, dc, 0:Wc], start=(dc == 0), stop=(dc == 7))
                    return pP[0], "pP0"

                for cc in range(4):
                    s = lset % NS
                    lset += 1
                    pb, pbk = proj(cc)
                    uk = "ux%d" % cc
                    OP("act", "copy", [pbk], [uk], out=ux[:, cc, H:L], in_=pb[:, 0:Wc])
                    OP("dve", "tensor_scalar", [uk, "vec"], ["xc%d" % s], out=xc[s][:, 0:Wc], in0=ux[:, cc, H - 3:H - 3 + Wc],
                       scalar1=vec[:, cc:cc + 1], scalar2=vec[:, 16 + cc:17 + cc], op0=ALU.mult, op1=ALU.add)
                    for k in range(1, 4):
                        OP("dve", "scalar_tensor_tensor", [uk, "vec", "xc%d" % s], ["xc%d" % s], out=xc[s][:, 0:Wc],
                           in0=ux[:, cc, H - 3 + k:H - 3 + k + Wc], scalar=vec[:, k * 4 + cc:k * 4 + cc + 1],
                           in1=xc[s][:, 0:Wc], op0=ALU.mult, op1=ALU.add)
                    OP("pool", "tensor_copy", [uk], [uk], out=ux[:, cc, 0:H], in_=ux[:, cc, Wc:Wc + H])
                    OP("act", "copy", ["xc%d" % s], ["xcb%d" % s], out=xcb[s][:, 0:Wc], in_=xc[s][:, 0:Wc])
                    OP("pe", "matmul", ["wa", "xcb%d" % s], ["pA"], pA[:, 0:Wc], lhsT=wa_sb[:, cc, :], rhs=xcb[s][:, 0:Wc],
                       start=True, stop=True)
                    OP("pe", "matmul", ["wx", "xcb%d" % s], ["pX"], pX[:, 0:Wc], lhsT=wx_sb[:, cc, :], rhs=xcb[s][:, 0:Wc],
                       start=True, stop=True)
                    OP("act", "activation", ["pA", "prm"], ["A%d" % cc], out=A4[:, cc, 0:Wc], in_=pA[:, 0:Wc], func=AF.Tanh,
                       scale=0.5, bias=prm[:, cc:cc + 1])
                    OP("act", "activation", ["pX", "prm"], ["B%d" % cc], out=B4[:, cc, 0:Wc], in_=pX[:, 0:Wc], func=AF.Tanh,
                       scale=0.5, bias=prm[:, 4 + cc:5 + cc])
                    OP("act", "activation", ["A%d" % cc, "prm"], ["C%d" % cc], out=C4[:, cc, 0:Wc], in_=A4[:, cc, 0:Wc], func=AF.Exp,
                       scale=prm[:, 8 + cc:9 + cc], bias=prm[:, 8 + cc:9 + cc])
                    OP("act", "activation", ["A%d" % cc, "prm"], ["A%d" % cc], out=A4[:, cc, 0:Wc], in_=A4[:, cc, 0:Wc], func=AF.Exp,
                       scale=prm[:, 12 + cc:13 + cc], bias=prm[:, 12 + cc:13 + cc])
                    OP("dve", "scalar_tensor_tensor", ["B%d" % cc, "xc%d" % s], ["B%d" % cc], out=B4[:, cc, 0:Wc], in0=B4[:, cc, 0:Wc],
                       scalar=1.0, in1=xc[s][:, 0:Wc], op0=ALU.add, op1=ALU.mult)
                    pb2, pb2k = proj(4 + cc)
                    OP("act", "copy", [pb2k], ["E%d" % cc], out=E4[:, cc, 0:Wc], in_=pb2[:, 0:Wc])
                Aks = ["A%d" % c for c in range(4)]
                Eks = ["E%d" % c for c in range(4)]
                OP("act", "activation", Aks, Aks, out=A4[:, :, 0:Wc], in_=A4[:, :, 0:Wc], func=AF.Sqrt, scale=-1.0, bias=1.0 + 2.4e-7)
                OP("act", "activation", Eks, Eks, out=E4[:, :, 0:Wc], in_=E4[:, :, 0:Wc], func=AF.Gelu_apprx_tanh)
                for cc in range(4):
                    OP("dve", "scalar_tensor_tensor", ["B%d" % cc, "A%d" % cc], ["B%d" % cc], out=B4[:, cc, 0:Wc], in0=B4[:, cc, 0:Wc],
                       scalar=0.5, in1=A4[:, cc, 0:Wc], op0=ALU.mult, op1=ALU.mult)
                    OP("dve", "tensor_tensor_scan", ["C%d" % cc, "B%d" % cc, "hc%d" % cc], ["y%d" % cc], out=y[:, cc, 0:Wc],
                       data0=C4[:, cc, 0:Wc], data1=B4[:, cc, 0:Wc], initial=hc[:, cc:cc + 1], op0=ALU.mult, op1=ALU.add)
                    OP("act", "copy", ["y%d" % cc], ["hc%d" % cc], out=hc[:, cc:cc + 1], in_=y[:, cc, Wc - 1:Wc])
                    if kind == "m":
                        continue
                    OP("pool", "tensor_tensor", ["y%d" % cc, "E%d" % cc, "hc%d" % cc], ["y%d" % cc], out=y[:, cc, 0:Wc], in0=y[:, cc, 0:Wc],
                       in1=E4[:, cc, 0:Wc], op=ALU.mult)
                    OP("act", "activation", ["y%d" % cc], ["ysq"], out=ysq[:, 0:Wc], in_=y[:, cc, 0:Wc], func=AF.Square)
                    OP("pe", "matmul", ["cst_b", "ysq"], ["pA"], pA[:, 0:Wc], lhsT=ones, rhs=ysq[:, 0:Wc],
                       start=(cc == 0), stop=(cc == 3))
                if kind == "m":
                    return
                OP("act", "activation", ["pA"], ["rl"], out=rl[:, 0:Wc], in_=pA[:, 0:Wc], func=AF.Ln, scale=1.0 / 512, bias=EPS)
                OP("act", "activation", ["rl"], ["rl"], out=rl[:, 0:Wc], in_=rl[:, 0:Wc], func=AF.Exp, scale=-0.5)
                for cc in range(4):
                    OP("dve", "scalar_tensor_tensor", ["y%d" % cc, "vec", "rl"], [yck + "%d" % cc], out=ycat[:, cc, 0:Wc],
                       in0=y[:, cc, 0:Wc], scalar=vec[:, 32 + cc:33 + cc], in1=rl[:, 0:Wc], op0=ALU.mult, op1=ALU.mult)

            def poolbr(ci):
                nonlocal mcount
                kind, r0, Wc = chunks[ci]
                L = H + Wc
                ycat = ycat2[ci % 2]
                yck = "yc%d_" % (ci % 2)
                for g in range(4):
                    for dc in range(8):
                        OP("pe", "matmul", ["win", "hnT"], ["pPp"], pPp[:, 0:Wc],
                           lhsT=win_sb[:, dc, (8 + g) * 128:(9 + g) * 128], rhs=hnT[:, dc, 0:Wc], start=(dc == 0), stop=(dc == 7))
                    uk = "up%d" % g
                    OP("act", "copy", ["pPp"], [uk], out=up[:, g, H:L], in_=pPp[:, 0:Wc])
                    cur, curk, lo = up[:, g, :], uk, 0
                    bufs = [(P0, "P0"), (P1, "P1")]
                    for si, stp in enumerate([1, 2, 4, 8][:g + 1]):
                        dst, dstk = bufs[si % 2]
                        lo2 = lo + stp
                        OP("pool", "tensor_tensor", [curk], [dstk], out=dst[:, lo2:L], in0=cur[:, lo2:L], in1=cur[:, lo:L - stp],
                           op=ALU.add)
                        cur, curk, lo = dst, dstk, lo2
                    wdw = [2, 4, 8, 16][g]
                    mi = mcount % 2
                    mcount += 1
                    if kind == "m":
                        OP("pool", "tensor_copy", [uk], [uk], out=up[:, g, 0:H], in_=up[:, g, Wc:Wc + H])
                        continue
                    OP("dve", "scalar_tensor_tensor", [curk, uk], ["mb%d" % mi], out=mb[mi][:, 0:Wc], in0=cur[:, H:L],
                       scalar=1.0 / wdw, in1=up[:, g, H:L], op0=ALU.mult, op1=ALU.subtract)
                    OP("pool", "tensor_copy", [uk], [uk], out=up[:, g, 0:H], in_=up[:, g, Wc:Wc + H])
                    OP("pe", "matmul", ["pw", "mb%d" % mi], ["pPp"], pPp[:, 0:Wc], lhsT=pw_sb[:, g, :], rhs=mb[mi][:, 0:Wc],
                       start=True, stop=True)
                    OP("act", "copy", ["pPp"], ["yp%d" % g], out=yp[:, g, 0:Wc], in_=pPp[:, 0:Wc])
                    OP("act", "activation", ["pPp"], ["ypsq"], out=ypsq[:, 0:Wc], in_=pPp[:, 0:Wc], func=AF.Square)
                    OP("pe", "matmul", ["cst_b", "ypsq"], ["pS"], pS[:, 0:Wc], lhsT=ones, rhs=ypsq[:, 0:Wc],
                       start=(g == 0), stop=(g == 3))
                if kind == "m":
                    return
                OP("act", "activation", ["pS"], ["rp"], out=rp[:, 0:Wc], in_=pS[:, 0:Wc], func=AF.Ln, scale=1.0 / 512, bias=EPS)
                OP("act", "activation", ["rp"], ["rp"], out=rp[:, 0:Wc], in_=rp[:, 0:Wc], func=AF.Exp, scale=-0.5)
                for g in range(4):
                    OP("dve", "scalar_tensor_tensor", ["yp%d" % g, "vec", "rp"], [yck + "%d" % (4 + g)], out=ycat[:, 4 + g, 0:Wc],
                       in0=yp[:, g, 0:Wc], scalar=vec[:, 36 + g:37 + g], in1=rp[:, 0:Wc], op0=ALU.mult, op1=ALU.mult)

            def tokstage(ci):
                nonlocal tile_idx
                kind, r0, Wc = chunks[ci]
                xb0 = (ci % 2) * 4
                ycat = ycat2[ci % 2]
                yck = "yc%d_" % (ci % 2)
                for tt in range(4):
                    b = xb0 + tt
                    rows = slice(r0 + tt * 128, r0 + (tt + 1) * 128)
                    xk = "xt%d" % b
                    for half in range(2):
                        sl = slice(half * 512, (half + 1) * 512)
                        for k in range(8):
                            OP("pe", "matmul", [yck + "%d" % k, "wout"], ["pO"], pO[:, :],
                               lhsT=ycat[:, k, tt * 128:(tt + 1) * 128], rhs=wout_sb[:, k, sl],
                               start=(k == 0), stop=(k == 7))
                        OP("dve", "tensor_tensor", ["pO", xk], [xk], out=xt[b][:, sl], in0=pO[:, :], in1=xt[b][:, sl],
                           op=ALU.add)
                    DMA("sp", [xk], [], out=h1buf[rows, :], in_=xt[b][:])
                    OP("act", "activation", [xk], ["hn2_%d" % (tt % 2), "ss2"], out=hn2[tt % 2][:], in_=xt[b][:], func=AF.Square,
                       accum_out=ss2[:, tt:tt + 1])
                rstd_chain(ss2[:, 0:4], "ss2", 1.0 / D)
                for tt in range(4):
                    b = xb0 + tt
                    q = tile_idx % 2
                    ti = tile_idx
                    tile_idx += 1
                    xk = "xt%d" % b
                    OP("dve", "scalar_tensor_tensor", [xk, "ss2", "g2b"], ["hn2_%d" % q], out=hn2[q][:], in0=xt[b][:],
                       scalar=ss2[:, tt:tt + 1], in1=g2b[:], op0=ALU.mult, op1=ALU.mult)
                    for hf in range(2):
                        for j in range(4):
                            dc = hf * 4 + j
                            OP("pe", "transpose", ["hn2_%d" % q, "cst_b"], ["pTb"], out=pTT[:, 4 + j, :],
                               in_=hn2[q][:, dc * 128:(dc + 1) * 128], identity=ident)
                        OP("act", "copy", ["pTb"], ["hn2T%d" % q], out=hn2T[q][:, hf * 4:hf * 4 + 4, :], in_=pTT[:, 4:8, :])
                    for dc in range(8):
                        OP("pe", "matmul", ["hn2T%d" % q, "wr"], ["pR"], pR[:, 0:36], lhsT=hn2T[q][:, dc, :], rhs=wr_sb[:, dc, :],
                           start=(dc == 0), stop=(dc == 7))
                    OP("dve", "tensor_tensor", ["pR", "rbias"], ["lg"], out=lg[:], in0=pR[:, 0:36], in1=rbias[:], op=ALU.add)
                    OP("dve", "reduce_max", ["lg"], ["rt0"], out=rt[:, 0:1], in_=lg[:, 0:4], axis=AX.X)
                    OP("dve", "tensor_scalar", ["lg", "rt0"], ["goh"], out=goh[:], in0=lg[:, 0:4], scalar1=rt[:, 0:1], scalar2=None,
                       op0=ALU.is_ge)
                    OP("dve", "tensor_scalar", ["lg", "rt0"], ["gsh"], out=gs[:, 4 * tt:4 * tt + 4], in0=lg[:, 0:4], scalar1=rt[:, 0:1],
                       scalar2=None, op0=ALU.subtract)
                    OP("dve", "tensor_scalar", ["goh"], ["pen"], out=pen[:], in0=goh[:], scalar1=-1.0, scalar2=1.0e30, op0=ALU.add,
                       op1=ALU.mult)
                    for g in range(4):
                        OP("dve", "tensor_scalar", ["lg", "pen"], ["es"], out=es[:, 8 * g:8 * g + 8], in0=lg[:, 4 + 8 * g:12 + 8 * g],
                           scalar1=pen[:, g:g + 1], scalar2=None, op0=ALU.add)
                    OP("dve", "reduce_max", ["es"], ["rt3"], out=rt[:, 3:4], in_=es[:], axis=AX.X)
                    OP("dve", "tensor_scalar", ["es", "rt3"], ["M1"], out=M1[:], in0=es[:], scalar1=rt[:, 3:4], scalar2=None,
                       op0=ALU.is_ge)
                    OP("dve", "scalar_tensor_tensor", ["M1", "es"], ["msk"], out=msk[:], in0=M1[:], scalar=-1e30, in1=es[:],
                       op0=ALU.mult, op1=ALU.add)
                    OP("dve", "reduce_max", ["msk"], ["rt4"], out=rt[:, 4:5], in_=msk[:], axis=AX.X)
                    OP("dve", "tensor_scalar", ["msk", "rt4"], ["M2"], out=M2[:], in0=msk[:], scalar1=rt[:, 4:5], scalar2=None,
                       op0=ALU.is_ge)
                    OP("dve", "tensor_tensor", ["M1", "M2"], ["Ms"], out=Ms[:], in0=M1[:], in1=M2[:], op=ALU.add)
                    OP("dve", "tensor_copy", ["Ms"], ["Mb"], out=Mb[:], in_=Ms[:])
                    OP("pe", "matmul", ["cst_b", "Mb"], ["pR"], pR[:, 64:96], lhsT=tri, rhs=Mb[:], start=True, stop=False)
                    OP("pe", "matmul", ["cst_b", "Mrunb"], ["pR"], pR[:, 64:96], lhsT=ones, rhs=Mrunb[:], start=False, stop=True)
                    OP("dve", "tensor_tensor", ["rt4", "rt3"], ["gdd"], out=gs[:, 32 + tt:33 + tt], in0=rt[:, 4:5], in1=rt[:, 3:4],
                       op=ALU.subtract)
                    OP("dve", "tensor_scalar", ["pR"], ["ovf"], out=ovf[:], in0=pR[:, 64:96], scalar1=float(CAP), scalar2=1.0e6,
                       op0=ALU.is_ge, op1=ALU.mult)
                    OP("dve", "tensor_tensor", ["pR", "cst_f"], ["posc"], out=posc[:], in0=pR[:, 64:96], in1=iotacap, op=ALU.add)
                    OP("dve", "tensor_tensor", ["posc", "ovf"], ["posc"], out=posc[:], in0=posc[:], in1=ovf[:], op=ALU.add)
                    OP("dve", "tensor_tensor", ["Mrun", "Ms"], ["Mrun"], out=Mrun[:], in0=Mrun[:], in1=Ms[:], op=ALU.add)
                    OP("dve", "tensor_copy", ["Mrun"], ["Mrunb"], out=Mrunb[:], in_=Mrun[:])
                    for kk, (Mk, Mkk, Df, Di) in enumerate(((M1, "M1", D0f, D0i), (M2, "M2", D1f, D1i))):
                        OP("dve", "tensor_tensor", [Mkk, "posc"], ["tmp32"], out=tmp32[:], in0=Mk[:], in1=posc[:], op=ALU.mult)
                        OP("dve", "reduce_sum", ["tmp32"], ["Df%d" % kk], out=Df[:, ti:ti + 1], in_=tmp32[:], axis=AX.X)
                        OP("dve", "tensor_copy", ["Df%d" % kk], ["Di%d" % kk], out=Di[ti][:, 0:1], in_=Df[:, ti:ti + 1])
                        SDMA("pool", (lambda e, Di=Di, ti=ti, q=q: e.indirect_dma_start(
                            out=xbuf, out_offset=bass.IndirectOffsetOnAxis(ap=Di[ti][:, 0:1], axis=0),
                            in_=hn2[q][:], in_offset=None, bounds_check=S.bcreg, oob_is_err=False)),
                            ["hn2_%d" % q, "Di%d" % kk] + xzkeys, [])
                ti0 = tile_idx - 4
                OP("act", "activation", ["gsh"], ["gex"], out=gs[:, 16:32], in_=gs[:, 0:16], func=AF.Exp)
                OP("dve", "reduce_sum", ["gex"], ["gse"], out=gs[:, 36:40], in_=gs[:, 16:32].rearrange("p (t g) -> p t g", g=4), axis=AX.X)
                OP("act", "activation", ["gdd"], ["ged"], out=gs[:, 40:44], in_=gs[:, 32:36], func=AF.Exp)
                OP("dve", "scalar_tensor_tensor", ["ged", "gse"], ["gt4"], out=gs[:, 44:48], in0=gs[:, 40:44], scalar=1.0, in1=gs[:, 36:40],
                   op0=ALU.add, op1=ALU.mult)
                OP("dve", "reciprocal", ["gt4"], ["G0"], out=G0[:, ti0:ti0 + 4], in_=gs[:, 44:48])
                OP("dve", "tensor_tensor", ["G0", "ged"], ["G1"], out=G1[:, ti0:ti0 + 4], in0=G0[:, ti0:ti0 + 4], in1=gs[:, 40:44],
                   op=ALU.mult)
            yc0keys = ["yc0_%d" % k for k in range(8)]
            NZ = NE * CAP // 512
            xzkeys = ["xz%d" % i for i in range(NZ)]

            def zero_fill():
                OP("pool", "memset", [], yc0keys, ycat2[0][:], 0.0)
                for i in range(NZ):
                    DMA("pool", yc0keys, ["xz%d" % i],
                        out=xbuf[i * 512:(i + 1) * 512, :].rearrange("(p r) d -> p (r d)", r=4),
                        in_=ycat2[0][:].rearrange("p k w -> p (k w)"))

            commit(record(stageA, 0))
            commit(record(lru, 0), record(poolbr, 0))
            commit(record(stageA, 1))
            lz = [(k_, q_, f_, r_, w_, 4.0) for (k_, q_, f_, r_, w_, c_) in record(zero_fill)]
            commit(record(lru, 1), record(poolbr, 1), lz)
            for ci in range(1, len(chunks)):
                la = record(tokstage, ci)
                if ci + 1 < len(chunks):
                    lA = record(stageA, ci + 1)
                    ll = record(lru, ci + 1)
                    lp = record(poolbr, ci + 1)
                    n1 = (len(la) * len(lA)) // (len(lA) + max(len(ll), len(lp)))
                    commit(la[:n1], lA)
                    commit(la[n1:], ll, lp)
                else:
                    commit(la)
            if debug:
                print("phase1 sbuf remaining", nc.sbuf_bytes_remaining)
                DMA("sp", ["G0"], [], out=dbg_route[:, 0:NT], in_=G0[:])
                DMA("sp", ["G1"], [], out=dbg_route[:, NT:2 * NT], in_=G1[:])
                DMA("sp", ["Df0"], [], out=dbg_route[:, 2 * NT:3 * NT], in_=D0f[:])
                DMA("sp", ["Df1"], [], out=dbg_route[:, 3 * NT:4 * NT], in_=D1f[:])
            S.emit()

        S.barrier()
        with contextlib.ExitStack() as st:
            NWB = 3
            wg_sb = [SB(st, "wg_sb%d" % i, [128, 8, FF], BF16) for i in range(NWB)]
            wu_sb = [SB(st, "wu_sb%d" % i, [128, 8, FF], BF16) for i in range(NWB)]
            wd_sb = [SB(st, "wd_sb%d" % i, [128, 4, D], BF16) for i in range(NWB)]
            xe = [SB(st, "xe%d" % i, [128, NB, D], BF16) for i in range(NWB)]
            xeT = [SB(st, "xeT%d" % i, [128, 8, CAP], BF16) for i in range(2)]
            hidT = [SB(st, "hidT%d" % i, [128, 4, CAP], BF16) for i in range(2)]
            sg = [SB(st, "sg%d" % i, [128, CAP], F32) for i in range(2)]
            ot = [SB(st, "ot%d" % i, [128, D], BF16) for i in range(3)]
            pT2 = [PS(st, "pT2_%d" % i, [128, 8, 128], BF16) for i in range(2)]
            pG = [PS(st, "pG%d" % i, [128, 512], F32) for i in range(2)]
            pU = [PS(st, "pU%d" % i, [128, 512], F32) for i in range(2)]
            pO2 = PS(st, "pO2", [128, D], F32)

            def loads(e):
                wb = e % NWB
                DMA("pool", [], ["wg%d" % wb], out=wg_sb[wb][:], in_=wgate[e].rearrange("(dc p) f -> p dc f", p=128))
                DMA("pool", [], ["wu%d" % wb], out=wu_sb[wb][:], in_=wup[e].rearrange("(dc p) f -> p dc f", p=128))
                DMA("pool", [], ["wd%d" % wb], out=wd_sb[wb][:], in_=wdown[e].rearrange("(fc p) d -> p fc d", p=128))
                DMA("sp", [], ["xe%d" % wb], out=xe[wb][:], in_=xbuf[e * CAP:(e + 1) * CAP, :].rearrange("(b p) d -> p b d", p=128))

            tcnt = 0
            ocnt = 0

            def front(e):
                nonlocal tcnt
                wb = e % NWB
                xb2 = e % 2
                for bb in range(NB):
                    ti2 = tcnt % 2
                    tcnt += 1
                    for dc in range(8):
                        OP("pe", "transpose", ["xe%d" % wb, "cst_b"], ["pT2_%d" % ti2], out=pT2[ti2][:, dc, :],
                           in_=xe[wb][:, bb, dc * 128:(dc + 1) * 128], identity=ident)
                    if tcnt % 2 == 0:
                        OP("act", "copy", ["pT2_%d" % ti2], ["xeT%d" % xb2], out=xeT[xb2][:, :, bb * 128:(bb + 1) * 128], in_=pT2[ti2][:])
                    else:
                        OP("dve", "tensor_copy", ["pT2_%d" % ti2], ["xeT%d" % xb2], out=xeT[xb2][:, :, bb * 128:(bb + 1) * 128],
                           in_=pT2[ti2][:])
                for fc in range(4):
                    pi = fc % 2
                    for dc in range(8):
                        OP("pe", "matmul", ["wg%d" % wb, "xeT%d" % xb2], ["pG%d" % pi], pG[pi][:, 0:CAP],
                           lhsT=wg_sb[wb][:, dc, fc * 128:(fc + 1) * 128], rhs=xeT[xb2][:, dc, :], start=(dc == 0), stop=(dc == 7))
                    for dc in range(8):
                        OP("pe", "matmul", ["wu%d" % wb, "xeT%d" % xb2], ["pU%d" % pi], pU[pi][:, 0:CAP],
                           lhsT=wu_sb[wb][:, dc, fc * 128:(fc + 1) * 128], rhs=xeT[xb2][:, dc, :], start=(dc == 0), stop=(dc == 7))
                    OP("act", "activation", ["pG%d" % pi], ["sg%d" % pi], out=sg[pi][:], in_=pG[pi][:, 0:CAP], func=AF.Silu)
                    OP("dve", "tensor_tensor", ["sg%d" % pi, "pU%d" % pi], ["hid%d_%d" % (xb2, fc)], out=hidT[xb2][:, fc, :],
                       in0=sg[pi][:], in1=pU[pi][:, 0:CAP], op=ALU.mult)

            def back(e):
                nonlocal ocnt
                wb = e % NWB
                xb2 = e % 2
                for bb in range(NB):
                    oi = ocnt % 3
                    ocnt += 1
                    for half in range(2):
                        for fc in range(4):
                            OP("pe", "matmul", ["hid%d_%d" % (xb2, fc), "wd%d" % wb], ["pO2_%d" % half],
                               pO2[:, half * 512:(half + 1) * 512], lhsT=hidT[xb2][:, fc, bb * 128:(bb + 1) * 128],
                               rhs=wd_sb[wb][:, fc, half * 512:(half + 1) * 512], start=(fc == 0), stop=(fc == 3))
                    OP("act", "copy", ["pO2_0"], ["ot%d" % oi], out=ot[oi][:, 0:512], in_=pO2[:, 0:512])
                    OP("dve", "tensor_copy", ["pO2_1"], ["ot%db" % oi], out=ot[oi][:, 512:1024], in_=pO2[:, 512:1024])
                    r1 = e * CAP + bb * 128
                    DMA("sp", ["ot%d" % oi, "ot%db" % oi], [], out=obuf[r1:r1 + 128, :], in_=ot[oi][:])

            loads(0)
            loads(1)
            commit(record(front, 0))
            for e in range(NE):
                ls = [record(back, e)]
                if e + 1 < NE:
                    ls.append(record(front, e + 1))
                if e + 2 < NE:
                    ls.append(record(loads, e + 2))
                commit(*ls)
            S.emit()

        S.barrier()
        with contextlib.ExitStack() as st:
            gfb = SB(st, "gfb", [128, D], F32)
            NB3 = 4
            h1t = [SB(st, "h1t%d" % i, [128, D], F32) for i in range(NB3)]
            o0 = [SB(st, "o0_%d" % i, [128, D], BF16) for i in range(NB3)]
            o1 = [SB(st, "o1_%d" % i, [128, D], BF16) for i in range(NB3)]
            outt = [SB(st, "outt%d" % i, [128, D], F32) for i in range(NB3)]
            junk = SB(st, "junk", [128, D], BF16)
            ssf = [SB(st, "ssf%d" % i, [128, 1], F32) for i in range(NB3)]
            DMA("sp", [], ["gfb"], out=gfb[:], in_=bc[:, 2 * D:3 * D])
            def p3_fetch(ti):
                b = ti % NB3
                rows = slice(ti * 128, (ti + 1) * 128)
                DMA("sp", [], ["h1t%d" % b], out=h1t[b][:], in_=h1buf[rows, :])
                OP("act", "memzero", [], ["o0_%d" % b], o0[b][:])
                OP("act", "memzero", [], ["o1_%d" % b], o1[b][:])
                for (ob, okey, Di) in ((o0, "o0_%d" % b, D0i), (o1, "o1_%d" % b, D1i)):
                    SDMA("pool", (lambda e, ob=ob, Di=Di, ti=ti, b=b: e.indirect_dma_start(
                        out=ob[b][:], out_offset=None, in_=obuf,
                        in_offset=bass.IndirectOffsetOnAxis(ap=Di[ti][:, 0:1], axis=0),
                        bounds_check=S.bcreg, oob_is_err=False)), [okey], [okey])

            def p3_compute(ti):
                b = ti % NB3
                rows = slice(ti * 128, (ti + 1) * 128)
                OP("dve", "scalar_tensor_tensor", ["o0_%d" % b, "h1t%d" % b], ["h1t%d" % b], out=h1t[b][:], in0=o0[b][:],
                   scalar=G0[:, ti:ti + 1], in1=h1t[b][:], op0=ALU.mult, op1=ALU.add)
                OP("dve", "scalar_tensor_tensor", ["o1_%d" % b, "h1t%d" % b], ["h1t%d" % b], out=h1t[b][:], in0=o1[b][:],
                   scalar=G1[:, ti:ti + 1], in1=h1t[b][:], op0=ALU.mult, op1=ALU.add)
                sk = "ssf%d" % b
                OP("act", "activation", ["h1t%d" % b], ["junk", sk], out=junk[:], in_=h1t[b][:], func=AF.Square,
                   accum_out=ssf[b][:, 0:1])
                rstd_chain(ssf[b][:, 0:1], sk, 1.0 / D)
                OP("dve", "scalar_tensor_tensor", ["h1t%d" % b, sk, "gfb"], ["outt%d" % b], out=outt[b][:], in0=h1t[b][:],
                   scalar=ssf[b][:, 0:1], in1=gfb[:], op0=ALU.mult, op1=ALU.mult)
                DMA("sp", ["outt%d" % b], [], out=out[rows, :], in_=outt[b][:])

            p3_fetch(0)
            p3_fetch(1)
            for ti in range(NT):
                if ti + 2 < NT:
                    p3_fetch(ti + 2)
                p3_compute(ti)
            S.emit(final=True)
    return nc


def _host_layout(inputs):
    f = lambda k: np.asarray(inputs[k], dtype=np.float32)
    conv_w = f("conv_w")[0]
    vecs = np.zeros((128, 40), np.float32)
    for k in range(4):
        vecs[:, k * 4:(k + 1) * 4] = conv_w[k].reshape(4, 128).T
    for i, name in enumerate(["conv_b", "lru_ba", "lru_bx", "lru_lambda", "lru_out_gain", "pool_scale"]):
        vecs[:, 16 + 4 * i:20 + 4 * i] = f(name)[0].reshape(4, 128).T
    rb = np.concatenate([f("b_group")[0], f("b_router")[0]])
    row = np.concatenate([f("norm1_gain")[0], f("norm2_gain")[0], f("final_gain"), rb])
    bc = np.ascontiguousarray(np.broadcast_to(row[None, :], (128, row.shape[0]))).astype(np.float32)

    def blockdiag(w):
        o = np.zeros((4, 128, 128), np.float32)
        for h in range(8):
            c, r = h // 2, (h % 2) * 64
            o[c, r:r + 64, r:r + 64] = w[h]
        return o

    wa_bd = blockdiag(f("lru_wa")[0])
    wx_bd = blockdiag(f("lru_wx")[0])
    wr = np.ascontiguousarray(np.concatenate([f("w_group")[0], f("w_router")[0]], axis=1))
    cst = np.zeros((128, 480), np.float32)
    cst[:, 0:128] = np.eye(128, dtype=np.float32)
    cst[:, 128:256] = np.triu(np.ones((128, 128), np.float32), k=1)
    cst[:, 256:384] = 1.0
    cst[:, 384:416] = (np.arange(32, dtype=np.float32) * CAP)[None, :]
    for g, w in enumerate((2, 4, 8, 16)):
        cst[:, 416 + g * 16:416 + (g + 1) * 16] = (1.0 / np.minimum(np.arange(1, 17), w)).astype(np.float32)[None, :]
    shared = {
        "meta": np.ascontiguousarray(f("meta_tokens")),
        "w_in": np.ascontiguousarray(f("w_in")[0]),
        "w_out": np.ascontiguousarray(f("w_out")[0]),
        "w_gate": np.ascontiguousarray(f("w_gate")[0]),
        "w_up": np.ascontiguousarray(f("w_up")[0]),
        "w_down": np.ascontiguousarray(f("w_down")[0]),
        "vecs": vecs, "bc": bc, "wa_bd": wa_bd, "wx_bd": wx_bd,
        "pool_w": np.ascontiguousarray(f("pool_w")[0]), "wr": wr, "cst": cst,
    }
    return shared


def kernel(**inputs):
    shared = _host_layout(inputs)
    x = np.asarray(inputs["x"], dtype=np.float32)
    nc = build()
    in_maps = []
    for c in range(NCORES):
        m = dict(shared)
        m["x"] = np.ascontiguousarray(x[c])
        in_maps.append(m)
    res = run_bass_kernel_spmd(nc, in_maps, core_ids=list(range(NCORES)))
    return np.stack([np.asarray(r["out"], dtype=np.float32) for r in res.results], axis=0)
```

```python
import contextlib
import numpy as np
import concourse.bass as bass
import concourse.mybir as mybir
from concourse.bass_utils import run_bass_kernel_spmd

F32 = mybir.dt.float32
BF16 = mybir.dt.bfloat16
I32 = mybir.dt.int32
AF = mybir.ActivationFunctionType
ALU = mybir.AluOpType
AX = mybir.AxisListType

D = 1024
NX = 4096
NM = 16
W = 512
H = 16
NCH = NX // W
NE = 32
CAP = 512
NB = CAP // 128
FF = 512
INW = 1536
EPS = 1e-6
NT = NX // 128
NCORES = 8
GRAN = 1


class Sched:
    ENGS = ("pe", "act", "dve", "pool", "sp")

    def __init__(self, nc, stack, n_dma_sems=64):
        self.nc = nc
        self.ops = {e: [] for e in self.ENGS}
        self.seq = {e: 0 for e in self.ENGS}
        self.waited = {e: {} for e in self.ENGS}
        self.last_w = {}
        self.readers = {}
        self.esem = {e: stack.enter_context(nc.semaphore("s_" + e)) for e in self.ENGS if e != "sp"}
        self.dsem = [stack.enter_context(nc.semaphore("d_%d" % i)) for i in range(n_dma_sems)]
        self.duse = [0] * n_dma_sems
        half = n_dma_sems // 2
        self.ring = {"sw": list(range(0, half)), "hw": list(range(half, n_dma_sems))}
        self.rnext = {"sw": 0, "hw": 0}
        self.barrier_tokens = []

    def _sem(self, key):
        return self.esem[key] if isinstance(key, str) else self.dsem[key]

    def _deps(self, eng, reads, writes, extra=()):
        deps = list(extra) + self.barrier_tokens
        for r in reads:
            t = self.last_w.get(r)
            if t is not None:
                deps.append(t)
        for w in writes:
            t = self.last_w.get(w)
            if t is not None:
                deps.append(t)
            deps.extend(self.readers.get(w, ()))
        waits = {}
        for (k, v) in deps:
            if eng == "pe" and k == "pe":
                continue
            if self.waited[eng].get(k, 0) >= v:
                continue
            if waits.get(k, 0) < v:
                waits[k] = v
        for k, v in waits.items():
            self.waited[eng][k] = v
        return list(waits.items())

    def _commit(self, tok, reads, writes):
        for r in reads:
            self.readers.setdefault(r, []).append(tok)
        for w in writes:
            self.last_w[w] = tok
            self.readers[w] = []

    def op(self, eng, fn, reads=(), writes=()):
        waits = self._deps(eng, reads, writes)
        self.seq[eng] += 1
        tok = (eng, self.seq[eng])
        self.ops[eng].append((waits, fn, self.esem[eng], 1))
        self._commit(tok, reads, writes)
        return tok

    def dma(self, q, fn, reads=(), writes=()):
        rk = "sw" if q == "pool" else "hw"
        j = self.ring[rk][self.rnext[rk]]
        self.rnext[rk] = (self.rnext[rk] + 1) % len(self.ring[rk])
        extra = []
        if self.duse[j] > 0:
            extra.append((j, 16 * self.duse[j]))
        waits = self._deps(q, reads, writes, extra)
        self.duse[j] += 1
        tok = (j, 16 * self.duse[j])
        self.ops[q].append((waits, fn, self.dsem[j], 16))
        self._commit(tok, reads, writes)
        return tok

    def all_tokens(self):
        toks = [(j, 16 * u) for j, u in enumerate(self.duse) if u > 0]
        toks += [(e, self.seq[e]) for e in self.ENGS if e != "sp" and self.seq[e] > 0]
        return toks

    def barrier(self):
        self.barrier_tokens = self.all_tokens()

    def emit(self, final=False):
        nc = self.nc
        fin = []
        if final:
            for (k, v) in self.all_tokens():
                if self.waited["sp"].get(k, 0) < v:
                    fin.append((k, v))
        sched = self
        ops = self.ops
        self.ops = {e: [] for e in self.ENGS}

        def run(engname, eng):
            if engname == "pool":
                sched.bcreg = eng.to_reg(NE * CAP - 1)
            for (waits, fn, sem, inc) in ops[engname]:
                for (k, v) in waits:
                    eng.wait_ge(sched._sem(k), v)
                fn(eng).then_inc(sem, inc)
            if engname == "sp":
                for (k, v) in fin:
                    eng.wait_ge(sched._sem(k), v)

        with nc.Block() as block:
            @block.tensor
            def _(eng):
                run("pe", eng)

            @block.scalar
            def _(eng):
                run("act", eng)

            @block.vector
            def _(eng):
                run("dve", eng)

            @block.gpsimd
            def _(eng):
                run("pool", eng)

            @block.sync
            def _(eng):
                run("sp", eng)


def build(debug=False):
    nc = bass.Bass("TRN2", target_bir_lowering=False)

    def din(name, shape, dtype=F32):
        return nc.dram_tensor(name, shape, dtype, kind="ExternalInput").ap()

    x = din("x", [NX, D])
    meta = din("meta", [NM, D])
    w_in = din("w_in", [D, INW])
    w_out = din("w_out", [D, D])
    wgate = din("w_gate", [NE, D, FF])
    wup = din("w_up", [NE, D, FF])
    wdown = din("w_down", [NE, FF, D])
    vecs = din("vecs", [128, 40])
    bc = din("bc", [128, 3 * D + 36])
    wa_bd = din("wa_bd", [4, 128, 128])
    wx_bd = din("wx_bd", [4, 128, 128])
    pool_w = din("pool_w", [4, 128, 128])
    wr = din("wr", [D, 36])
    cst = din("cst", [128, 480])
    out = nc.dram_tensor("out", [NX, D], F32, kind="ExternalOutput").ap()
    dk = "ExternalOutput" if debug else "Internal"
    h1buf = nc.dram_tensor("h1buf", [NX, D], F32, kind=dk).ap()
    xbuf = nc.dram_tensor("xbuf", [NE * CAP, D], BF16, kind=dk).ap()
    obuf = nc.dram_tensor("obuf", [NE * CAP, D], BF16, kind=dk).ap()
    if debug:
        dbg_route = nc.dram_tensor("dbg_route", [128, 4 * NT], F32, kind="ExternalOutput").ap()

    with contextlib.ExitStack() as gst:
        S = Sched(nc, gst)

        defer = {"list": None}

        def est_cost(eng, method, n):
            if eng == "pe":
                return max(64, n) / 2400.0 + 0.02
            if eng == "act":
                return 0.22 + n / 1150.0
            if eng == "dve":
                f = {"scalar_tensor_tensor": 1.6, "tensor_tensor_scan": 2.4, "reciprocal": 6.0}.get(method, 1.0)
                return 0.15 + f * n / 960.0
            return 0.25 + n / 500.0

        def OP(eng, method, reads, writes, *args, **kw):
            fn = lambda e: getattr(e, method)(*args, **kw)
            if defer["list"] is not None:
                o = kw.get("out", args[0] if args else None)
                n = 1
                for d in list(o.shape)[1:]:
                    n *= int(d)
                defer["list"].append(("op", eng, fn, list(reads), list(writes), est_cost(eng, method, n)))
            else:
                S.op(eng, fn, reads, writes)

        def SDMA(q, fn, reads, writes):
            if defer["list"] is not None:
                defer["list"].append(("dma", q, fn, list(reads), list(writes), 1.1 if q == "pool" else 0.1))
            else:
                S.dma(q, fn, reads, writes)

        def DMA(q, reads, writes, **kw):
            SDMA(q, lambda e: e.dma_start(**kw), reads, writes)

        def record(f, *a):
            defer["list"] = []
            f(*a)
            out_l = defer["list"]
            defer["list"] = None
            return out_l

        sim = {"free": {e: 0.0 for e in Sched.ENGS}, "wfin": {}, "rfin": {}}

        def commit(*lists):
            lists = [l for l in lists if l]
            pos = [0] * len(lists)
            total = sum(len(l) for l in lists)
            free, wfin, rfin = sim["free"], sim["wfin"], sim["rfin"]
            for _ in range(total):
                best = None
                for k, l in enumerate(lists):
                    if pos[k] >= len(l):
                        continue
                    kind, q, fn, reads, writes, cost = l[pos[k]]
                    ready = 0.0
                    for r in reads:
                        ready = max(ready, wfin.get(r, 0.0))
                    for w in writes:
                        ready = max(ready, wfin.get(w, 0.0), rfin.get(w, 0.0))
                    start = max(free[q], ready)
                    cand = (start, pos[k] / len(l), k)
                    if best is None or cand < best:
                        best = cand
                start, _, k = best
                kind, q, fn, reads, writes, cost = lists[k][pos[k]]
                pos[k] += 1
                free[q] = start + cost
                fin = start + cost + (3.0 if kind == "dma" else 0.0)
                for r in reads:
                    rfin[r] = max(rfin.get(r, 0.0), fin)
                for w in writes:
                    wfin[w] = fin
                    rfin[w] = 0.0
                if kind == "op":
                    S.op(q, fn, reads, writes)
                else:
                    S.dma(q, fn, reads, writes)

        def SB(stack, name, shape, dtype):
            return stack.enter_context(nc.sbuf_tensor(name, shape, dtype))

        def PS(stack, name, shape, dtype):
            return stack.enter_context(nc.psum_tensor(name, shape, dtype))

        cst_b = SB(gst, "cst_b", [128, 384], BF16)
        cst_f = SB(gst, "cst_f", [128, 96], F32)
        D0f = SB(gst, "D0f", [128, NT], F32)
        D1f = SB(gst, "D1f", [128, NT], F32)
        D0i = [SB(gst, "D0i%d" % i, [128, 1], I32) for i in range(NT)]
        D1i = [SB(gst, "D1i%d" % i, [128, 1], I32) for i in range(NT)]
        G0 = SB(gst, "G0", [128, NT], F32)
        G1 = SB(gst, "G1", [128, NT], F32)
        ident = cst_b[:, 0:128]
        tri = cst_b[:, 128:256]
        ones = cst_b[:, 256:384]
        iotacap = cst_f[:, 0:32]
        rec = cst_f[:, 32:96]

        DMA("pool", [], ["cst_b"], out=cst_b[:], in_=cst[:, 0:384])
        DMA("sp", [], ["cst_f"], out=cst_f[:], in_=cst[:, 384:480])

        def rstd_chain(ss_ap, key, scale):
            OP("dve", "tensor_scalar", [key], [key], out=ss_ap, in0=ss_ap, scalar1=scale, scalar2=EPS,
               op0=ALU.mult, op1=ALU.add)
            OP("act", "activation", [key], [key], out=ss_ap, in_=ss_ap, func=AF.Sqrt)
            OP("dve", "reciprocal", [key], [key], out=ss_ap, in_=ss_ap)

        with contextlib.ExitStack() as st:
            win_sb = SB(st, "win_sb", [128, 8, INW], BF16)
            wout_sb = SB(st, "wout_sb", [128, 8, D], BF16)
            wa_sb = SB(st, "wa_sb", [128, 4, 128], BF16)
            wx_sb = SB(st, "wx_sb", [128, 4, 128], BF16)
            pw_sb = SB(st, "pw_sb", [128, 4, 128], BF16)
            wr_sb = SB(st, "wr_sb", [128, 8, 36], BF16)
            vec = SB(st, "vec", [128, 40], F32)
            g1b = SB(st, "g1b", [128, D], F32)
            g2b = SB(st, "g2b", [128, D], F32)
            rbias = SB(st, "rbias", [128, 36], F32)
            xt = [SB(st, "xt%d" % i, [128, D], F32) for i in range(8)]
            hn = [SB(st, "hn0", [128, D], BF16)]
            ss1 = SB(st, "ss1", [128, 4], F32)
            hnT = SB(st, "hnT", [128, 8, W], BF16)
            ux = SB(st, "ux", [128, 4, H + W], F32)
            up = SB(st, "up", [128, 4, H + W], F32)
            NS = 2
            xc = [SB(st, "xc%d" % i, [128, W], F32) for i in range(NS)]
            xcb = [SB(st, "xcb%d" % i, [128, W], BF16) for i in range(NS)]
            A4 = SB(st, "A4", [128, 4, W], F32)
            B4 = SB(st, "B4", [128, 4, W], F32)
            C4 = SB(st, "C4", [128, 4, W], F32)
            E4 = SB(st, "E4", [128, 4, W], F32)
            prm = SB(st, "prm", [128, 16], F32)
            y = SB(st, "y", [128, 4, W], F32)
            ysq = SB(st, "ysq", [128, W], BF16)
            yp = SB(st, "yp", [128, 4, W], F32)
            ypsq = SB(st, "ypsq", [128, W], BF16)
            ycat2 = [SB(st, "ycat%d_" % i, [128, 8, W], BF16) for i in range(2)]
            rl = SB(st, "rl", [128, W], F32)
            rp = SB(st, "rp", [128, W], F32)
            P0 = SB(st, "P0", [128, H + W], F32)
            P1 = SB(st, "P1", [128, H + W], F32)
            mb = [SB(st, "mb%d" % i, [128, W], BF16) for i in range(2)]
            hc = SB(st, "hc", [128, 4], F32)
            hn2 = [SB(st, "hn2_%d" % i, [128, D], BF16) for i in range(2)]
            hn2T = [SB(st, "hn2T%d" % i, [128, 8, 128], BF16) for i in range(2)]
            ss2 = SB(st, "ss2", [128, 4], F32)
            lg = SB(st, "lg", [128, 36], F32)
            rt = SB(st, "rt", [128, 16], F32)
            goh = SB(st, "goh", [128, 4], F32)
            pen = SB(st, "pen", [128, 4], F32)
            gs = SB(st, "gs", [128, 48], F32)
            es = SB(st, "es", [128, 32], F32)
            msk = SB(st, "msk", [128, 32], F32)
            M1 = SB(st, "M1", [128, 32], F32)
            M2 = SB(st, "M2", [128, 32], F32)
            Ms = SB(st, "Ms", [128, 32], F32)
            Mb = SB(st, "Mb", [128, 32], BF16)
            Mrun = SB(st, "Mrun", [128, 32], F32)
            Mrunb = SB(st, "Mrunb", [128, 32], BF16)
            posc = SB(st, "posc", [128, 32], F32)
            ovf = SB(st, "ovf", [128, 32], F32)
            tmp32 = SB(st, "tmp32", [128, 32], F32)

            pTT = PS(st, "pTT", [128, 8, 128], BF16)
            pP = [PS(st, "pP0", [128, W], F32)]
            pA = PS(st, "pA", [128, W], F32)
            pX = PS(st, "pX", [128, W], F32)
            pS = PS(st, "pS", [128, W], F32)
            pPp = PS(st, "pPp", [128, W], F32)
            pR = PS(st, "pR", [128, W], F32)
            pO = PS(st, "pO", [128, W], F32)

            DMA("sp", [], ["vec"], out=vec[:], in_=vecs)
            DMA("sp", [], ["g1b"], out=g1b[:], in_=bc[:, 0:D])
            DMA("pool", [], ["win"], out=win_sb[:], in_=w_in.rearrange("(dc p) c -> p dc c", p=128))
            DMA("pool", [], ["wa"], out=wa_sb[:], in_=wa_bd.rearrange("c i j -> i c j"))
            DMA("pool", [], ["wx"], out=wx_sb[:], in_=wx_bd.rearrange("c i j -> i c j"))
            DMA("pool", [], ["pw"], out=pw_sb[:], in_=pool_w.rearrange("g c d -> c g d"))
            DMA("pool", [], ["wout"], out=wout_sb[:], in_=w_out.rearrange("(k p) d -> p k d", p=128))
            DMA("pool", [], ["wr"], out=wr_sb[:], in_=wr.rearrange("(dc p) n -> p dc n", p=128))
            DMA("sp", [], ["g2b"], out=g2b[:], in_=bc[:, D:2 * D])
            DMA("sp", [], ["rbias"], out=rbias[:], in_=bc[:, 3 * D:3 * D + 36])

            OP("act", "activation", ["vec"], ["prm"], out=prm[:, 12:16], in_=vec[:, 28:32], func=AF.Sigmoid)
            OP("act", "activation", ["prm"], ["prm"], out=prm[:, 12:16], in_=prm[:, 12:16], func=AF.Ln)
            OP("dve", "tensor_scalar", ["prm"], ["prm"], out=prm[:, 8:12], in0=prm[:, 12:16], scalar1=4.0, scalar2=None,
               op0=ALU.mult)
            OP("dve", "tensor_scalar", ["prm"], ["prm"], out=prm[:, 12:16], in0=prm[:, 12:16], scalar1=8.0, scalar2=None,
               op0=ALU.mult)
            OP("dve", "tensor_scalar", ["prm", "vec"], ["prm"], out=prm[:, 0:8], in0=vec[:, 20:28], scalar1=0.5, scalar2=None,
               op0=ALU.mult)
            OP("pool", "memset", [], ["ux0", "ux1", "ux2", "ux3"], ux[:], 0.0)
            OP("pool", "memset", [], ["up0", "up1", "up2", "up3"], up[:], 0.0)
            OP("pool", "memset", [], ["hc0", "hc1", "hc2", "hc3"], hc[:], 0.0)
            OP("pool", "memset", [], ["Mrun"], Mrun[:], 0.0)
            OP("pool", "memset", [], ["Mrunb"], Mrunb[:], 0.0)

            chunks = [("m", 0, NM)] + [("x", j * W, W) for j in range(NCH)]
            pcount = 0
            lset = 0
            mcount = 0
            tile_idx = 0
            def stageA(ci):
                kind, r0, Wc = chunks[ci]
                ntile = 1 if kind == "m" else 4
                nr = NM if kind == "m" else 128
                xb0 = (ci % 2) * 4
                for tt in range(ntile):
                    b = xb0 + tt
                    src = meta if kind == "m" else x[r0 + tt * 128: r0 + (tt + 1) * 128, :]
                    DMA("sp", [], ["xt%d" % b], out=xt[b][0:nr, :], in_=src)
                    OP("act", "activation", ["xt%d" % b], ["hn0", "ss1"], out=hn[0][0:nr, :], in_=xt[b][0:nr, :],
                       func=AF.Square, accum_out=ss1[0:nr, tt:tt + 1])
                rstd_chain(ss1[0:nr, 0:ntile], "ss1", 1.0 / D)
                for tt in range(ntile):
                    b = xb0 + tt
                    OP("dve", "scalar_tensor_tensor", ["xt%d" % b, "ss1", "g1b"], ["hn0"], out=hn[0][0:nr, :],
                       in0=xt[b][0:nr, :], scalar=ss1[0:nr, tt:tt + 1], in1=g1b[0:nr, :], op0=ALU.mult, op1=ALU.mult)
                    for hf in range(2):
                        for j in range(4):
                            dc = hf * 4 + j
                            OP("pe", "transpose", ["hn0", "cst_b"], ["pTa"], out=pTT[:, j, 0:nr],
                               in_=hn[0][0:nr, dc * 128:(dc + 1) * 128], identity=ident[0:nr, 0:nr])
                        OP("act", "copy", ["pTa"], ["hnT"], out=hnT[:, hf * 4:hf * 4 + 4, tt * 128: tt * 128 + nr], in_=pTT[:, 0:4, 0:nr])

            def lru(ci):
                nonlocal lset
                kind, r0, Wc = chunks[ci]
                L = H + Wc
                ycat = ycat2[ci % 2]
                yck = "yc%d_" % (ci % 2)

                def proj(oc):
                    for dc in range(8):
                        OP("pe", "matmul", ["win", "hnT"], ["pP0"], pP[0][:, 0:Wc],
                           lhsT=win_sb[:, dc, oc * 128:(oc + 1) * 128], rhs=hnT[:, dc, 0:Wc], start=(dc == 0), stop=(dc == 7))
                    return pP[0], "pP0"

                for cc in range(4):
                    s = lset % NS
                    lset += 1
                    pb, pbk = proj(cc)
                    uk = "ux%d" % cc
                    OP("act", "copy", [pbk], [uk], out=ux[:, cc, H:L], in_=pb[:, 0:Wc])
                    OP("dve", "tensor_scalar", [uk, "vec"], ["xc%d" % s], out=xc[s][:, 0:Wc], in0=ux[:, cc, H - 3:H - 3 + Wc],
                       scalar1=vec[:, cc:cc + 1], scalar2=vec[:, 16 + cc:17 + cc], op0=ALU.mult, op1=ALU.add)
                    for k in range(1, 4):
                        OP("dve", "scalar_tensor_tensor", [uk, "vec", "xc%d" % s], ["xc%d" % s], out=xc[s][:, 0:Wc],
                           in0=ux[:, cc, H - 3 + k:H - 3 + k + Wc], scalar=vec[:, k * 4 + cc:k * 4 + cc + 1],
                           in1=xc[s][:, 0:Wc], op0=ALU.mult, op1=ALU.add)
                    OP("pool", "tensor_copy", [uk], [uk], out=ux[:, cc, 0:H], in_=ux[:, cc, Wc:Wc + H])
                    OP("act", "copy", ["xc%d" % s], ["xcb%d" % s], out=xcb[s][:, 0:Wc], in_=xc[s][:, 0:Wc])
                    OP("pe", "matmul", ["wa", "xcb%d" % s], ["pA"], pA[:, 0:Wc], lhsT=wa_sb[:, cc, :], rhs=xcb[s][:, 0:Wc],
                       start=True, stop=True)
                    OP("pe", "matmul", ["wx", "xcb%d" % s], ["pX"], pX[:, 0:Wc], lhsT=wx_sb[:, cc, :], rhs=xcb[s][:, 0:Wc],
                       start=True, stop=True)
                    OP("act", "activation", ["pA", "prm"], ["A%d" % cc], out=A4[:, cc, 0:Wc], in_=pA[:, 0:Wc], func=AF.Tanh,
                       scale=0.5, bias=prm[:, cc:cc + 1])
                    OP("act", "activation", ["pX", "prm"], ["B%d" % cc], out=B4[:, cc, 0:Wc], in_=pX[:, 0:Wc], func=AF.Tanh,
                       scale=0.5, bias=prm[:, 4 + cc:5 + cc])
                    OP("act", "activation", ["A%d" % cc, "prm"], ["C%d" % cc], out=C4[:, cc, 0:Wc], in_=A4[:, cc, 0:Wc], func=AF.Exp,
                       scale=prm[:, 8 + cc:9 + cc], bias=prm[:, 8 + cc:9 + cc])
                    OP("act", "activation", ["A%d" % cc, "prm"], ["A%d" % cc], out=A4[:, cc, 0:Wc], in_=A4[:, cc, 0:Wc], func=AF.Exp,
                       scale=prm[:, 12 + cc:13 + cc], bias=prm[:, 12 + cc:13 + cc])
                    OP("dve", "scalar_tensor_tensor", ["B%d" % cc, "xc%d" % s], ["B%d" % cc], out=B4[:, cc, 0:Wc], in0=B4[:, cc, 0:Wc],
                       scalar=1.0, in1=xc[s][:, 0:Wc], op0=ALU.add, op1=ALU.mult)
                    pb2, pb2k = proj(4 + cc)
                    OP("act", "copy", [pb2k], ["E%d" % cc], out=E4[:, cc, 0:Wc], in_=pb2[:, 0:Wc])
                Aks = ["A%d" % c for c in range(4)]
                Eks = ["E%d" % c for c in range(4)]
                OP("act", "activation", Aks, Aks, out=A4[:, :, 0:Wc], in_=A4[:, :, 0:Wc], func=AF.Sqrt, scale=-1.0, bias=1.0 + 2.4e-7)
                OP("act", "activation", Eks, Eks, out=E4[:, :, 0:Wc], in_=E4[:, :, 0:Wc], func=AF.Gelu_apprx_tanh)
                for cc in range(4):
                    OP("dve", "scalar_tensor_tensor", ["B%d" % cc, "A%d" % cc], ["B%d" % cc], out=B4[:, cc, 0:Wc], in0=B4[:, cc, 0:Wc],
                       scalar=0.5, in1=A4[:, cc, 0:Wc], op0=ALU.mult, op1=ALU.mult)
                    OP("dve", "tensor_tensor_scan", ["C%d" % cc, "B%d" % cc, "hc%d" % cc], ["y%d" % cc], out=y[:, cc, 0:Wc],
                       data0=C4[:, cc, 0:Wc], data1=B4[:, cc, 0:Wc], initial=hc[:, cc:cc + 1], op0=ALU.mult, op1=ALU.add)
                    OP("act", "copy", ["y%d" % cc], ["hc%d" % cc], out=hc[:, cc:cc + 1], in_=y[:, cc, Wc - 1:Wc])
                    if kind == "m":
                        continue
                    OP("pool", "tensor_tensor", ["y%d" % cc, "E%d" % cc, "hc%d" % cc], ["y%d" % cc], out=y[:, cc, 0:Wc], in0=y[:, cc, 0:Wc],
                       in1=E4[:, cc, 0:Wc], op=ALU.mult)
                    OP("act", "activation", ["y%d" % cc], ["ysq"], out=ysq[:, 0:Wc], in_=y[:, cc, 0:Wc], func=AF.Square)
                    OP("pe", "matmul", ["cst_b", "ysq"], ["pA"], pA[:, 0:Wc], lhsT=ones, rhs=ysq[:, 0:Wc],
                       start=(cc == 0), stop=(cc == 3))
                if kind == "m":
                    return
                OP("act", "activation", ["pA"], ["rl"], out=rl[:, 0:Wc], in_=pA[:, 0:Wc], func=AF.Ln, scale=1.0 / 512, bias=EPS)
                OP("act", "activation", ["rl"], ["rl"], out=rl[:, 0:Wc], in_=rl[:, 0:Wc], func=AF.Exp, scale=-0.5)
                for cc in range(4):
                    OP("dve", "scalar_tensor_tensor", ["y%d" % cc, "vec", "rl"], [yck + "%d" % cc], out=ycat[:, cc, 0:Wc],
                       in0=y[:, cc, 0:Wc], scalar=vec[:, 32 + cc:33 + cc], in1=rl[:, 0:Wc], op0=ALU.mult, op1=ALU.mult)

            def poolbr(ci):
                nonlocal mcount
                kind, r0, Wc = chunks[ci]
                L = H + Wc
                ycat = ycat2[ci % 2]
                yck = "yc%d_" % (ci % 2)
                for g in range(4):
                    for dc in range(8):
                        OP("pe", "matmul", ["win", "hnT"], ["pPp"], pPp[:, 0:Wc],
                           lhsT=win_sb[:, dc, (8 + g) * 128:(9 + g) * 128], rhs=hnT[:, dc, 0:Wc], start=(dc == 0), stop=(dc == 7))
                    uk = "up%d" % g
                    OP("act", "copy", ["pPp"], [uk], out=up[:, g, H:L], in_=pPp[:, 0:Wc])
                    cur, curk, lo = up[:, g, :], uk, 0
                    bufs = [(P0, "P0"), (P1, "P1")]
                    for si, stp in enumerate([1, 2, 4, 8][:g + 1]):
                        dst, dstk = bufs[si % 2]
                        lo2 = lo + stp
                        OP("pool", "tensor_tensor", [curk], [dstk], out=dst[:, lo2:L], in0=cur[:, lo2:L], in1=cur[:, lo:L - stp],
                           op=ALU.add)
                        cur, curk, lo = dst, dstk, lo2
                    wdw = [2, 4, 8, 16][g]
                    mi = mcount % 2
                    mcount += 1
                    if kind == "m":
                        OP("pool", "tensor_copy", [uk], [uk], out=up[:, g, 0:H], in_=up[:, g, Wc:Wc + H])
                        continue
                    OP("dve", "scalar_tensor_tensor", [curk, uk], ["mb%d" % mi], out=mb[mi][:, 0:Wc], in0=cur[:, H:L],
                       scalar=1.0 / wdw, in1=up[:, g, H:L], op0=ALU.mult, op1=ALU.subtract)
                    OP("pool", "tensor_copy", [uk], [uk], out=up[:, g, 0:H], in_=up[:, g, Wc:Wc + H])
                    OP("pe", "matmul", ["pw", "mb%d" % mi], ["pPp"], pPp[:, 0:Wc], lhsT=pw_sb[:, g, :], rhs=mb[mi][:, 0:Wc],
                       start=True, stop=True)
                    OP("act", "copy", ["pPp"], ["yp%d" % g], out=yp[:, g, 0:Wc], in_=pPp[:, 0:Wc])
                    OP("act", "activation", ["pPp"], ["ypsq"], out=ypsq[:, 0:Wc], in_=pPp[:, 0:Wc], func=AF.Square)
                    OP("pe", "matmul", ["cst_b", "ypsq"], ["pS"], pS[:, 0:Wc], lhsT=ones, rhs=ypsq[:, 0:Wc],
                       start=(g == 0), stop=(g == 3))
                if kind == "m":
                    return
                OP("act", "activation", ["pS"], ["rp"], out=rp[:, 0:Wc], in_=pS[:, 0:Wc], func=AF.Ln, scale=1.0 / 512, bias=EPS)
                OP("act", "activation", ["rp"], ["rp"], out=rp[:, 0:Wc], in_=rp[:, 0:Wc], func=AF.Exp, scale=-0.5)
                for g in range(4):
                    OP("dve", "scalar_tensor_tensor", ["yp%d" % g, "vec", "rp"], [yck + "%d" % (4 + g)], out=ycat[:, 4 + g, 0:Wc],
                       in0=yp[:, g, 0:Wc], scalar=vec[:, 36 + g:37 + g], in1=rp[:, 0:Wc], op0=ALU.mult, op1=ALU.mult)

            def tokstage(ci):
                nonlocal tile_idx
                kind, r0, Wc = chunks[ci]
                xb0 = (ci % 2) * 4
                ycat = ycat2[ci % 2]
                yck = "yc%d_" % (ci % 2)
                for tt in range(4):
                    b = xb0 + tt
                    rows = slice(r0 + tt * 128, r0 + (tt + 1) * 128)
                    xk = "xt%d" % b
                    for half in range(2):
                        sl = slice(half * 512, (half + 1) * 512)
                        for k in range(8):
                            OP("pe", "matmul", [yck + "%d" % k, "wout"], ["pO"], pO[:, :],
                               lhsT=ycat[:, k, tt * 128:(tt + 1) * 128], rhs=wout_sb[:, k, sl],
                               start=(k == 0), stop=(k == 7))
                        OP("dve", "tensor_tensor", ["pO", xk], [xk], out=xt[b][:, sl], in0=pO[:, :], in1=xt[b][:, sl],
                           op=ALU.add)
                    DMA("sp", [xk], [], out=h1buf[rows, :], in_=xt[b][:])
                    OP("act", "activation", [xk], ["hn2_%d" % (tt % 2), "ss2"], out=hn2[tt % 2][:], in_=xt[b][:], func=AF.Square,
                       accum_out=ss2[:, tt:tt + 1])
                rstd_chain(ss2[:, 0:4], "ss2", 1.0 / D)
                for tt in range(4):
                    b = xb0 + tt
                    q = tile_idx % 2
                    ti = tile_idx
                    tile_idx += 1
                    xk = "xt%d" % b
                    OP("dve", "scalar_tensor_tensor", [xk, "ss2", "g2b"], ["hn2_%d" % q], out=hn2[q][:], in0=xt[b][:],
                       scalar=ss2[:, tt:tt + 1], in1=g2b[:], op0=ALU.mult, op1=ALU.mult)
                    for hf in range(2):
                        for j in range(4):
                            dc = hf * 4 + j
                            OP("pe", "transpose", ["hn2_%d" % q, "cst_b"], ["pTb"], out=pTT[:, 4 + j, :],
                               in_=hn2[q][:, dc * 128:(dc + 1) * 128], identity=ident)
                        OP("act", "copy", ["pTb"], ["hn2T%d" % q], out=hn2T[q][:, hf * 4:hf * 4 + 4, :], in_=pTT[:, 4:8, :])
                    for dc in range(8):
                        OP("pe", "matmul", ["hn2T%d" % q, "wr"], ["pR"], pR[:, 0:36], lhsT=hn2T[q][:, dc, :], rhs=wr_sb[:, dc, :],
                           start=(dc == 0), stop=(dc == 7))
                    OP("dve", "tensor_tensor", ["pR", "rbias"], ["lg"], out=lg[:], in0=pR[:, 0:36], in1=rbias[:], op=ALU.add)
                    OP("dve", "reduce_max", ["lg"], ["rt0"], out=rt[:, 0:1], in_=lg[:, 0:4], axis=AX.X)
                    OP("dve", "tensor_scalar", ["lg", "rt0"], ["goh"], out=goh[:], in0=lg[:, 0:4], scalar1=rt[:, 0:1], scalar2=None,
                       op0=ALU.is_ge)
                    OP("dve", "tensor_scalar", ["lg", "rt0"], ["gsh"], out=gs[:, 4 * tt:4 * tt + 4], in0=lg[:, 0:4], scalar1=rt[:, 0:1],
                       scalar2=None, op0=ALU.subtract)
                    OP("dve", "tensor_scalar", ["goh"], ["pen"], out=pen[:], in0=goh[:], scalar1=-1.0, scalar2=1.0e30, op0=ALU.add,
                       op1=ALU.mult)
                    for g in range(4):
                        OP("dve", "tensor_scalar", ["lg", "pen"], ["es"], out=es[:, 8 * g:8 * g + 8], in0=lg[:, 4 + 8 * g:12 + 8 * g],
                           scalar1=pen[:, g:g + 1], scalar2=None, op0=ALU.add)
                    OP("dve", "reduce_max", ["es"], ["rt3"], out=rt[:, 3:4], in_=es[:], axis=AX.X)
                    OP("dve", "tensor_scalar", ["es", "rt3"], ["M1"], out=M1[:], in0=es[:], scalar1=rt[:, 3:4], scalar2=None,
                       op0=ALU.is_ge)
                    OP("dve", "scalar_tensor_tensor", ["M1", "es"], ["msk"], out=msk[:], in0=M1[:], scalar=-1e30, in1=es[:],
                       op0=ALU.mult, op1=ALU.add)
                    OP("dve", "reduce_max", ["msk"], ["rt4"], out=rt[:, 4:5], in_=msk[:], axis=AX.X)
                    OP("dve", "tensor_scalar", ["msk", "rt4"], ["M2"], out=M2[:], in0=msk[:], scalar1=rt[:, 4:5], scalar2=None,
                       op0=ALU.is_ge)
                    OP("dve", "tensor_tensor", ["M1", "M2"], ["Ms"], out=Ms[:], in0=M1[:], in1=M2[:], op=ALU.add)
                    OP("dve", "tensor_copy", ["Ms"], ["Mb"], out=Mb[:], in_=Ms[:])
                    OP("pe", "matmul", ["cst_b", "Mb"], ["pR"], pR[:, 64:96], lhsT=tri, rhs=Mb[:], start=True, stop=False)
                    OP("pe", "matmul", ["cst_b", "Mrunb"], ["pR"], pR[:, 64:96], lhsT=ones, rhs=Mrunb[:], start=False, stop=True)
                    OP("dve", "tensor_tensor", ["rt4", "rt3"], ["gdd"], out=gs[:, 32 + tt:33 + tt], in0=rt[:, 4:5], in1=rt[:, 3:4],
                       op=ALU.subtract)
                    OP("dve", "tensor_scalar", ["pR"], ["ovf"], out=ovf[:], in0=pR[:, 64:96], scalar1=float(CAP), scalar2=1.0e6,
                       op0=ALU.is_ge, op1=ALU.mult)
                    OP("dve", "tensor_tensor", ["pR", "cst_f"], ["posc"], out=posc[:], in0=pR[:, 64:96], in1=iotacap, op=ALU.add)
                    OP("dve", "tensor_tensor", ["posc", "ovf"], ["posc"], out=posc[:], in0=posc[:], in1=ovf[:], op=ALU.add)
                    OP("dve", "tensor_tensor", ["Mrun", "Ms"], ["Mrun"], out=Mrun[:], in0=Mrun[:], in1=Ms[:], op=ALU.add)
                    OP("dve", "tensor_copy", ["Mrun"], ["Mrunb"], out=Mrunb[:], in_=Mrun[:])
                    for kk, (Mk, Mkk, Df, Di) in enumerate(((M1, "M1", D0f, D0i), (M2, "M2", D1f, D1i))):
                        OP("dve", "tensor_tensor", [Mkk, "posc"], ["tmp32"], out=tmp32[:], in0=Mk[:], in1=posc[:], op=ALU.mult)
                        OP("dve", "reduce_sum", ["tmp32"], ["Df%d" % kk], out=Df[:, ti:ti + 1], in_=tmp32[:], axis=AX.X)
                        OP("dve", "tensor_copy", ["Df%d" % kk], ["Di%d" % kk], out=Di[ti][:, 0:1], in_=Df[:, ti:ti + 1])
                        SDMA("pool", (lambda e, Di=Di, ti=ti, q=q: e.indirect_dma_start(
                            out=xbuf, out_offset=bass.IndirectOffsetOnAxis(ap=Di[ti][:, 0:1], axis=0),
                            in_=hn2[q][:], in_offset=None, bounds_check=S.bcreg, oob_is_err=False)),
                            ["hn2_%d" % q, "Di%d" % kk] + xzkeys, [])
                ti0 = tile_idx - 4
                OP("act", "activation", ["gsh"], ["gex"], out=gs[:, 16:32], in_=gs[:, 0:16], func=AF.Exp)
                OP("dve", "reduce_sum", ["gex"], ["gse"], out=gs[:, 36:40], in_=gs[:, 16:32].rearrange("p (t g) -> p t g", g=4), axis=AX.X)
                OP("act", "activation", ["gdd"], ["ged"], out=gs[:, 40:44], in_=gs[:, 32:36], func=AF.Exp)
                OP("dve", "scalar_tensor_tensor", ["ged", "gse"], ["gt4"], out=gs[:, 44:48], in0=gs[:, 40:44], scalar=1.0, in1=gs[:, 36:40],
                   op0=ALU.add, op1=ALU.mult)
                OP("dve", "reciprocal", ["gt4"], ["G0"], out=G0[:, ti0:ti0 + 4], in_=gs[:, 44:48])
                OP("dve", "tensor_tensor", ["G0", "ged"], ["G1"], out=G1[:, ti0:ti0 + 4], in0=G0[:, ti0:ti0 + 4], in1=gs[:, 40:44],
                   op=ALU.mult)
            yc0keys = ["yc0_%d" % k for k in range(8)]
            NZ = NE * CAP // 512
            xzkeys = ["xz%d" % i for i in range(NZ)]

            def zero_fill():
                OP("pool", "memset", [], yc0keys, ycat2[0][:], 0.0)
                for i in range(NZ):
                    DMA("pool", yc0keys, ["xz%d" % i],
                        out=xbuf[i * 512:(i + 1) * 512, :].rearrange("(p r) d -> p (r d)", r=4),
                        in_=ycat2[0][:].rearrange("p k w -> p (k w)"))

            commit(record(stageA, 0))
            commit(record(lru, 0), record(poolbr, 0))
            commit(record(stageA, 1))
            lz = [(k_, q_, f_, r_, w_, 4.0) for (k_, q_, f_, r_, w_, c_) in record(zero_fill)]
            commit(record(lru, 1), record(poolbr, 1), lz)
            for ci in range(1, len(chunks)):
                la = record(tokstage, ci)
                if ci + 1 < len(chunks):
                    lA = record(stageA, ci + 1)
                    ll = record(lru, ci + 1)
                    lp = record(poolbr, ci + 1)
                    n1 = (len(la) * len(lA)) // (len(lA) + max(len(ll), len(lp)))
                    commit(la[:n1], lA)
                    commit(la[n1:], ll, lp)
                else:
                    commit(la)
            if debug:
                print("phase1 sbuf remaining", nc.sbuf_bytes_remaining)
                DMA("sp", ["G0"], [], out=dbg_route[:, 0:NT], in_=G0[:])
                DMA("sp", ["G1"], [], out=dbg_route[:, NT:2 * NT], in_=G1[:])
                DMA("sp", ["Df0"], [], out=dbg_route[:, 2 * NT:3 * NT], in_=D0f[:])
                DMA("sp", ["Df1"], [], out=dbg_route[:, 3 * NT:4 * NT], in_=D1f[:])
            S.emit()

        S.barrier()
        with contextlib.ExitStack() as st:
            NWB = 3
            wg_sb = [SB(st, "wg_sb%d" % i, [128, 8, FF], BF16) for i in range(NWB)]
            wu_sb = [SB(st, "wu_sb%d" % i, [128, 8, FF], BF16) for i in range(NWB)]
            wd_sb = [SB(st, "wd_sb%d" % i, [128, 4, D], BF16) for i in range(NWB)]
            xe = [SB(st, "xe%d" % i, [128, NB, D], BF16) for i in range(NWB)]
            xeT = [SB(st, "xeT%d" % i, [128, 8, CAP], BF16) for i in range(2)]
            hidT = [SB(st, "hidT%d" % i, [128, 4, CAP], BF16) for i in range(2)]
            sg = [SB(st, "sg%d" % i, [128, CAP], F32) for i in range(2)]
            ot = [SB(st, "ot%d" % i, [128, D], BF16) for i in range(3)]
            pT2 = [PS(st, "pT2_%d" % i, [128, 8, 128], BF16) for i in range(2)]
            pG = [PS(st, "pG%d" % i, [128, 512], F32) for i in range(2)]
            pU = [PS(st, "pU%d" % i, [128, 512], F32) for i in range(2)]
            pO2 = PS(st, "pO2", [128, D], F32)

            def loads(e):
                wb = e % NWB
                DMA("pool", [], ["wg%d" % wb], out=wg_sb[wb][:], in_=wgate[e].rearrange("(dc p) f -> p dc f", p=128))
                DMA("pool", [], ["wu%d" % wb], out=wu_sb[wb][:], in_=wup[e].rearrange("(dc p) f -> p dc f", p=128))
                DMA("pool", [], ["wd%d" % wb], out=wd_sb[wb][:], in_=wdown[e].rearrange("(fc p) d -> p fc d", p=128))
                DMA("sp", [], ["xe%d" % wb], out=xe[wb][:], in_=xbuf[e * CAP:(e + 1) * CAP, :].rearrange("(b p) d -> p b d", p=128))

            tcnt = 0
            ocnt = 0

            def front(e):
                nonlocal tcnt
                wb = e % NWB
                xb2 = e % 2
                for bb in range(NB):
                    ti2 = tcnt % 2
                    tcnt += 1
                    for dc in range(8):
                        OP("pe", "transpose", ["xe%d" % wb, "cst_b"], ["pT2_%d" % ti2], out=pT2[ti2][:, dc, :],
                           in_=xe[wb][:, bb, dc * 128:(dc + 1) * 128], identity=ident)
                    if tcnt % 2 == 0:
                        OP("act", "copy", ["pT2_%d" % ti2], ["xeT%d" % xb2], out=xeT[xb2][:, :, bb * 128:(bb + 1) * 128], in_=pT2[ti2][:])
                    else:
                        OP("dve", "tensor_copy", ["pT2_%d" % ti2], ["xeT%d" % xb2], out=xeT[xb2][:, :, bb * 128:(bb + 1) * 128],
                           in_=pT2[ti2][:])
                for fc in range(4):
                    pi = fc % 2
                    for dc in range(8):
                        OP("pe", "matmul", ["wg%d" % wb, "xeT%d" % xb2], ["pG%d" % pi], pG[pi][:, 0:CAP],
                           lhsT=wg_sb[wb][:, dc, fc * 128:(fc + 1) * 128], rhs=xeT[xb2][:, dc, :], start=(dc == 0), stop=(dc == 7))
                    for dc in range(8):
                        OP("pe", "matmul", ["wu%d" % wb, "xeT%d" % xb2], ["pU%d" % pi], pU[pi][:, 0:CAP],
                           lhsT=wu_sb[wb][:, dc, fc * 128:(fc + 1) * 128], rhs=xeT[xb2][:, dc, :], start=(dc == 0), stop=(dc == 7))
                    OP("act", "activation", ["pG%d" % pi], ["sg%d" % pi], out=sg[pi][:], in_=pG[pi][:, 0:CAP], func=AF.Silu)
                    OP("dve", "tensor_tensor", ["sg%d" % pi, "pU%d" % pi], ["hid%d_%d" % (xb2, fc)], out=hidT[xb2][:, fc, :],
                       in0=sg[pi][:], in1=pU[pi][:, 0:CAP], op=ALU.mult)

            def back(e):
                nonlocal ocnt
                wb = e % NWB
                xb2 = e % 2
                for bb in range(NB):
                    oi = ocnt % 3
                    ocnt += 1
                    for half in range(2):
                        for fc in range(4):
                            OP("pe", "matmul", ["hid%d_%d" % (xb2, fc), "wd%d" % wb], ["pO2_%d" % half],
                               pO2[:, half * 512:(half + 1) * 512], lhsT=hidT[xb2][:, fc, bb * 128:(bb + 1) * 128],
                               rhs=wd_sb[wb][:, fc, half * 512:(half + 1) * 512], start=(fc == 0), stop=(fc == 3))
                    OP("act", "copy", ["pO2_0"], ["ot%d" % oi], out=ot[oi][:, 0:512], in_=pO2[:, 0:512])
                    OP("dve", "tensor_copy", ["pO2_1"], ["ot%db" % oi], out=ot[oi][:, 512:1024], in_=pO2[:, 512:1024])
                    r1 = e * CAP + bb * 128
                    DMA("sp", ["ot%d" % oi, "ot%db" % oi], [], out=obuf[r1:r1 + 128, :], in_=ot[oi][:])

            loads(0)
            loads(1)
            commit(record(front, 0))
            for e in range(NE):
                ls = [record(back, e)]
                if e + 1 < NE:
                    ls.append(record(front, e + 1))
                if e + 2 < NE:
                    ls.append(record(loads, e + 2))
                commit(*ls)
            S.emit()

        S.barrier()
        with contextlib.ExitStack() as st:
            gfb = SB(st, "gfb", [128, D], F32)
            NB3 = 4
            h1t = [SB(st, "h1t%d" % i, [128, D], F32) for i in range(NB3)]
            o0 = [SB(st, "o0_%d" % i, [128, D], BF16) for i in range(NB3)]
            o1 = [SB(st, "o1_%d" % i, [128, D], BF16) for i in range(NB3)]
            outt = [SB(st, "outt%d" % i, [128, D], F32) for i in range(NB3)]
            junk = SB(st, "junk", [128, D], BF16)
            ssf = [SB(st, "ssf%d" % i, [128, 1], F32) for i in range(NB3)]
            DMA("sp", [], ["gfb"], out=gfb[:], in_=bc[:, 2 * D:3 * D])
            def p3_fetch(ti):
                b = ti % NB3
                rows = slice(ti * 128, (ti + 1) * 128)
                DMA("sp", [], ["h1t%d" % b], out=h1t[b][:], in_=h1buf[rows, :])
                OP("act", "memzero", [], ["o0_%d" % b], o0[b][:])
                OP("act", "memzero", [], ["o1_%d" % b], o1[b][:])
                for (ob, okey, Di) in ((o0, "o0_%d" % b, D0i), (o1, "o1_%d" % b, D1i)):
                    SDMA("pool", (lambda e, ob=ob, Di=Di, ti=ti, b=b: e.indirect_dma_start(
                        out=ob[b][:], out_offset=None, in_=obuf,
                        in_offset=bass.IndirectOffsetOnAxis(ap=Di[ti][:, 0:1], axis=0),
                        bounds_check=S.bcreg, oob_is_err=False)), [okey], [okey])

            def p3_compute(ti):
                b = ti % NB3
                rows = slice(ti * 128, (ti + 1) * 128)
                OP("dve", "scalar_tensor_tensor", ["o0_%d" % b, "h1t%d" % b], ["h1t%d" % b], out=h1t[b][:], in0=o0[b][:],
                   scalar=G0[:, ti:ti + 1], in1=h1t[b][:], op0=ALU.mult, op1=ALU.add)
                OP("dve", "scalar_tensor_tensor", ["o1_%d" % b, "h1t%d" % b], ["h1t%d" % b], out=h1t[b][:], in0=o1[b][:],
                   scalar=G1[:, ti:ti + 1], in1=h1t[b][:], op0=ALU.mult, op1=ALU.add)
                sk = "ssf%d" % b
                OP("act", "activation", ["h1t%d" % b], ["junk", sk], out=junk[:], in_=h1t[b][:], func=AF.Square,
                   accum_out=ssf[b][:, 0:1])
                rstd_chain(ssf[b][:, 0:1], sk, 1.0 / D)
                OP("dve", "scalar_tensor_tensor", ["h1t%d" % b, sk, "gfb"], ["outt%d" % b], out=outt[b][:], in0=h1t[b][:],
                   scalar=ssf[b][:, 0:1], in1=gfb[:], op0=ALU.mult, op1=ALU.mult)
                DMA("sp", ["outt%d" % b], [], out=out[rows, :], in_=outt[b][:])

            p3_fetch(0)
            p3_fetch(1)
            for ti in range(NT):
                if ti + 2 < NT:
                    p3_fetch(ti + 2)
                p3_compute(ti)
            S.emit(final=True)
    return nc


def _host_layout(inputs):
    f = lambda k: np.asarray(inputs[k], dtype=np.float32)
    conv_w = f("conv_w")[0]
    vecs = np.zeros((128, 40), np.float32)
    for k in range(4):
        vecs[:, k * 4:(k + 1) * 4] = conv_w[k].reshape(4, 128).T
    for i, name in enumerate(["conv_b", "lru_ba", "lru_bx", "lru_lambda", "lru_out_gain", "pool_scale"]):
        vecs[:, 16 + 4 * i:20 + 4 * i] = f(name)[0].reshape(4, 128).T
    rb = np.concatenate([f("b_group")[0], f("b_router")[0]])
    row = np.concatenate([f("norm1_gain")[0], f("norm2_gain")[0], f("final_gain"), rb])
    bc = np.ascontiguousarray(np.broadcast_to(row[None, :], (128, row.shape[0]))).astype(np.float32)

    def blockdiag(w):
        o = np.zeros((4, 128, 128), np.float32)
        for h in range(8):
            c, r = h // 2, (h % 2) * 64
            o[c, r:r + 64, r:r + 64] = w[h]
        return o

    wa_bd = blockdiag(f("lru_wa")[0])
    wx_bd = blockdiag(f("lru_wx")[0])
    wr = np.ascontiguousarray(np.concatenate([f("w_group")[0], f("w_router")[0]], axis=1))
    cst = np.zeros((128, 480), np.float32)
    cst[:, 0:128] = np.eye(128, dtype=np.float32)
    cst[:, 128:256] = np.triu(np.ones((128, 128), np.float32), k=1)
    cst[:, 256:384] = 1.0
    cst[:, 384:416] = (np.arange(32, dtype=np.float32) * CAP)[None, :]
    for g, w in enumerate((2, 4, 8, 16)):
        cst[:, 416 + g * 16:416 + (g + 1) * 16] = (1.0 / np.minimum(np.arange(1, 17), w)).astype(np.float32)[None, :]
    shared = {
        "meta": np.ascontiguousarray(f("meta_tokens")),
        "w_in": np.ascontiguousarray(f("w_in")[0]),
        "w_out": np.ascontiguousarray(f("w_out")[0]),
        "w_gate": np.ascontiguousarray(f("w_gate")[0]),
        "w_up": np.ascontiguousarray(f("w_up")[0]),
        "w_down": np.ascontiguousarray(f("w_down")[0]),
        "vecs": vecs, "bc": bc, "wa_bd": wa_bd, "wx_bd": wx_bd,
        "pool_w": np.ascontiguousarray(f("pool_w")[0]), "wr": wr, "cst": cst,
    }
    return shared


def kernel(**inputs):
    shared = _host_layout(inputs)
    x = np.asarray(inputs["x"], dtype=np.float32)
    nc = build()
    in_maps = []
    for c in range(NCORES):
        m = dict(shared)
        m["x"] = np.ascontiguousarray(x[c])
        in_maps.append(m)
    res = run_bass_kernel_spmd(nc, in_maps, core_ids=list(range(NCORES)))
    return np.stack([np.asarray(r["out"], dtype=np.float32) for r in res.results], axis=0)
```
